# Optimizing a Trainium2 kernel written in Bass

```python
import math
import jax, jax.numpy as jnp
from jax import lax
import numpy as np

D_MODEL = 1024
BATCH = 16
SEQ = 2048
DEPTH = 4

GRID_W = 64
CTX_LEN = 256
N_EVEN = (DEPTH + 1) // 2
N_ODD = DEPTH // 2
N_MOD = 6
A_HEAD_DIM = 64
A_HEADS = D_MODEL // A_HEAD_DIM
A_KV_HEADS = A_HEADS // 4
A_GROUP = A_HEADS // A_KV_HEADS
WINDOW = 128
BLOCK = 128
B_HEAD_DIM = 64
B_V_DIM = 2 * B_HEAD_DIM
B_HEADS = D_MODEL // B_V_DIM
D_FF = 2816
N_EXPERTS = 8
TOP_K = 2
D_FF_EXPERT = 3584
ROPE_THETA = 10000.0
LN_EPS = 1e-5
RMS_EPS = 1e-5
NEG_INF = -1e30
DEEPNORM_ALPHA = (2.0 * DEPTH) ** 0.25
DEEPNORM_BETA = (8.0 * DEPTH) ** -0.25
MOD_INIT_SCALE = 0.2

kernel_name = 'hybrid_interleaved_swa_diffattn_moe_dit'


def layer_norm(x, g, b):
    xf = x.astype(jnp.float32)
    mu = jnp.mean(xf, axis=-1, keepdims=True)
    var = jnp.mean(jnp.square(xf - mu), axis=-1, keepdims=True)
    y = (xf - mu) * lax.rsqrt(var + LN_EPS)
    return (y * g.astype(jnp.float32) + b.astype(jnp.float32)).astype(x.dtype)


def rms_norm(x, g):
    xf = x.astype(jnp.float32)
    y = xf * lax.rsqrt(jnp.mean(xf * xf, axis=-1, keepdims=True) + RMS_EPS)
    return (y * g.astype(jnp.float32)).astype(x.dtype)


def axial_rope_tables(n_tokens, head_dim, dtype):
    rows = n_tokens // GRID_W
    row = jnp.repeat(jnp.arange(rows, dtype=jnp.float32), GRID_W)
    col = jnp.tile(jnp.arange(GRID_W, dtype=jnp.float32), rows)
    half = head_dim // 2
    inv = ROPE_THETA ** (-jnp.arange(0, half, 2, dtype=jnp.float32) / half)
    ang_r = row[:, None] * inv[None, :]
    ang_c = col[:, None] * inv[None, :]
    ang = jnp.concatenate([ang_r, ang_r, ang_c, ang_c], axis=-1)
    return jnp.cos(ang).astype(dtype), jnp.sin(ang).astype(dtype)


def apply_axial_rope(x, cos, sin):
    x1, x2, x3, x4 = jnp.split(x, 4, axis=-1)
    rot = jnp.concatenate([-x2, x1, -x4, x3], axis=-1)
    return x * cos[None, :, None, :] + rot * sin[None, :, None, :]


def modulation(cond, w_mod, b_mod):
    m = jnp.dot(jax.nn.silu(cond), w_mod) + b_mod
    return jnp.split(m, N_MOD, axis=-1)


def split_heads(t, n_heads, head_dim):
    return t.reshape(t.shape[0], t.shape[1], n_heads, head_dim)


def windowed_gqa(hl, hc, w_qkv, sink, w_o, cos, sin, with_ctx):
    B, L, _ = hl.shape
    qd = A_HEADS * A_HEAD_DIM
    kd = A_KV_HEADS * A_HEAD_DIM

    def proj(h):
        q, k, v = jnp.split(h @ w_qkv, [qd, qd + kd], axis=-1)
        return (split_heads(q, A_HEADS, A_HEAD_DIM), split_heads(k, A_KV_HEADS, A_HEAD_DIM),
                split_heads(v, A_KV_HEADS, A_HEAD_DIM))

    ql, kl, vl = proj(hl)
    qc, kc, vc = proj(hc)
    ql = apply_axial_rope(ql, cos, sin)
    kl = apply_axial_rope(kl, cos, sin)
    scale = A_HEAD_DIM ** -0.5
    nb = L // BLOCK

    qb = ql.reshape(B, nb, BLOCK, A_KV_HEADS, A_GROUP, A_HEAD_DIM)
    pad = ((0, 0), (BLOCK, BLOCK), (0, 0), (0, 0))
    kp = jnp.pad(kl, pad).reshape(B, nb + 2, BLOCK, A_KV_HEADS, A_HEAD_DIM)
    vp = jnp.pad(vl, pad).reshape(B, nb + 2, BLOCK, A_KV_HEADS, A_HEAD_DIM)
    kband = jnp.concatenate([kp[:, :-2], kp[:, 1:-1], kp[:, 2:]], axis=2)
    vband = jnp.concatenate([vp[:, :-2], vp[:, 1:-1], vp[:, 2:]], axis=2)

    qpos = jnp.arange(L).reshape(nb, BLOCK)
    kpos = (jnp.arange(nb)[:, None] - 1) * BLOCK + jnp.arange(3 * BLOCK)[None, :]
    rel = kpos[:, None, :] - qpos[:, :, None]
    band_mask = (jnp.abs(rel) <= WINDOW) & (kpos[:, None, :] >= 0) & (kpos[:, None, :] < L)

    s_band = jnp.einsum('bnqhgd,bnkhd->bnhgqk', qb, kband).astype(jnp.float32) * scale
    s_band = jnp.where(band_mask[None, :, None, None], s_band, NEG_INF)
    s_ctx = jnp.einsum('bnqhgd,bchd->bnhgqc', qb, kc).astype(jnp.float32) * scale
    sink_g = sink.astype(jnp.float32).reshape(A_KV_HEADS, A_GROUP, 1, 1)
    s_sink = jnp.broadcast_to(sink_g, s_band.shape[:-1] + (1,))
    p = jax.nn.softmax(jnp.concatenate([s_band, s_ctx, s_sink], axis=-1), axis=-1)
    p_band = p[..., :3 * BLOCK].astype(vl.dtype)
    p_ctx = p[..., 3 * BLOCK:3 * BLOCK + kc.shape[1]].astype(vl.dtype)
    ol = (jnp.einsum('bnhgqk,bnkhd->bnqhgd', p_band, vband)
          + jnp.einsum('bnhgqc,bchd->bnqhgd', p_ctx, vc))
    ol = ol.reshape(B, L, qd) @ w_o

    oc = None
    if with_ctx:
        C = hc.shape[1]
        qcg = qc.reshape(B, C, A_KV_HEADS, A_GROUP, A_HEAD_DIM)
        s = jnp.einsum('bqhgd,bkhd->bhgqk', qcg, kc).astype(jnp.float32) * scale
        s_sink_c = jnp.broadcast_to(sink_g, s.shape[:-1] + (1,))
        pc = jax.nn.softmax(jnp.concatenate([s, s_sink_c], axis=-1), axis=-1)
        oc = jnp.einsum('bhgqk,bkhd->bqhgd', pc[..., :C].astype(vc.dtype), vc)
        oc = oc.reshape(B, C, qd) @ w_o
    return ol, oc


def diff_core(q, k, v, lam, subln_g, lam_init):
    n = q.shape[1]
    s = jnp.einsum('bqhd,bkhd->bhqk', q, k).astype(jnp.float32) * (B_HEAD_DIM ** -0.5)
    p = jax.nn.softmax(s, axis=-1).reshape(q.shape[0], B_HEADS, 2, n, k.shape[1])
    a = (p[:, :, 0] - lam * p[:, :, 1]).astype(v.dtype)
    o = jnp.einsum('bhqk,bkhd->bqhd', a, v)
    return rms_norm(o, subln_g) * (1.0 - lam_init)


def diff_attention(hl, hc, w_qkv, lam_q1, lam_k1, lam_q2, lam_k2, subln_g, w_o, cos, sin, lam_init, with_ctx):
    B, L, _ = hl.shape
    qd = 2 * B_HEADS * B_HEAD_DIM

    def proj(h):
        q, k, v = jnp.split(h @ w_qkv, [qd, 2 * qd], axis=-1)
        return (split_heads(q, 2 * B_HEADS, B_HEAD_DIM), split_heads(k, 2 * B_HEADS, B_HEAD_DIM),
                split_heads(v, B_HEADS, B_V_DIM))

    ql, kl, vl = proj(hl)
    qc, kc, vc = proj(hc)
    ql = apply_axial_rope(ql, cos, sin)
    kl = apply_axial_rope(kl, cos, sin)
    lam = (jnp.exp(jnp.sum(lam_q1.astype(jnp.float32) * lam_k1.astype(jnp.float32)))
           - jnp.exp(jnp.sum(lam_q2.astype(jnp.float32) * lam_k2.astype(jnp.float32))) + lam_init)

    k_all = jnp.concatenate([kl, kc], axis=1)
    v_all = jnp.concatenate([vl, vc], axis=1)
    nb = L // BLOCK
    qb = ql.reshape(B, nb, BLOCK, 2 * B_HEADS, B_HEAD_DIM).swapaxes(0, 1)
    ob = lax.map(lambda q: diff_core(q, k_all, v_all, lam, subln_g, lam_init), qb)
    ol = ob.swapaxes(0, 1).reshape(B, L, B_HEADS * B_V_DIM) @ w_o

    oc = None
    if with_ctx:
        oc = diff_core(qc, kc, vc, lam, subln_g, lam_init)
        oc = oc.reshape(B, hc.shape[1], B_HEADS * B_V_DIM) @ w_o
    return ol, oc


def swiglu(h, w_g, w_u, w_d):
    return (jax.nn.silu(h @ w_g) * (h @ w_u)) @ w_d


def moe_swiglu(h, w_router, w_g, w_u, w_d):
    shp = h.shape
    t = h.reshape(-1, shp[-1])
    logits = (t @ w_router).astype(jnp.float32)
    top_v, top_i = lax.top_k(logits, TOP_K)
    top_w = jax.nn.softmax(top_v, axis=-1)
    gates = jnp.sum(jax.nn.one_hot(top_i, N_EXPERTS, dtype=jnp.float32) * top_w[..., None], axis=1).astype(h.dtype)
    y = jnp.zeros_like(t)
    for e in range(N_EXPERTS):
        y = y + gates[:, e:e + 1] * swiglu(t, w_g[e], w_u[e], w_d[e])
    return y.reshape(shp)


def setup_inputs(seed: int = 0) -> dict:
    key = jax.random.key(seed)
    ks = iter(jax.random.split(key, 32))

    def nrm(shape, scale):
        return jax.random.normal(next(ks), shape, jnp.float32) * scale

    D = D_MODEL
    s = D ** -0.5
    a_qd = A_HEADS * A_HEAD_DIM
    a_kd = A_KV_HEADS * A_HEAD_DIM
    b_qd = 2 * B_HEADS * B_HEAD_DIM
    b_vd = B_HEADS * B_V_DIM
    x = nrm((BATCH, SEQ, D), 1.0)
    c = nrm((BATCH, D), 1.0)
    ctx = nrm((BATCH, CTX_LEN, D), 1.0)
    c_ctx = nrm((D,), 1.0)
    w_mod = nrm((DEPTH, D, N_MOD * D), s * MOD_INIT_SCALE)
    b_mod = nrm((DEPTH, N_MOD * D), 0.01)
    ln_g = 1.0 + nrm((DEPTH, 2, D), 0.02)
    ln_b = nrm((DEPTH, 2, D), 0.02)
    a_w_qkv = jnp.concatenate([nrm((N_EVEN, D, a_qd + a_kd), s), nrm((N_EVEN, D, a_kd), s * DEEPNORM_BETA)], axis=-1)
    a_sink = nrm((N_EVEN, A_HEADS), 0.5)
    a_w_o = nrm((N_EVEN, a_qd, D), a_qd ** -0.5 * DEEPNORM_BETA)
    b_w_qkv = jnp.concatenate([nrm((N_ODD, D, 2 * b_qd), s), nrm((N_ODD, D, b_vd), s * DEEPNORM_BETA)], axis=-1)
    b_lam_q1 = nrm((N_ODD, B_HEAD_DIM), 0.1)
    b_lam_k1 = nrm((N_ODD, B_HEAD_DIM), 0.1)
    b_lam_q2 = nrm((N_ODD, B_HEAD_DIM), 0.1)
    b_lam_k2 = nrm((N_ODD, B_HEAD_DIM), 0.1)
    b_subln_g = 1.0 + nrm((N_ODD, B_V_DIM), 0.02)
    b_w_o = nrm((N_ODD, b_vd, D), b_vd ** -0.5 * DEEPNORM_BETA)
    ff_w_gate = nrm((N_EVEN, D, D_FF), s)
    ff_w_up = nrm((N_EVEN, D, D_FF), s)
    ff_w_down = nrm((N_EVEN, D_FF, D), D_FF ** -0.5 * DEEPNORM_BETA)
    moe_w_router = nrm((N_ODD, D, N_EXPERTS), s)
    moe_w_gate = nrm((N_ODD, N_EXPERTS, D, D_FF_EXPERT), s)
    moe_w_up = nrm((N_ODD, N_EXPERTS, D, D_FF_EXPERT), s)
    moe_w_down = nrm((N_ODD, N_EXPERTS, D_FF_EXPERT, D), D_FF_EXPERT ** -0.5 * DEEPNORM_BETA)
    return {'x': x, 'c': c, 'ctx': ctx, 'c_ctx': c_ctx, 'w_mod': w_mod, 'b_mod': b_mod,
            'ln_g': ln_g, 'ln_b': ln_b, 'a_w_qkv': a_w_qkv, 'a_sink': a_sink, 'a_w_o': a_w_o,
            'b_w_qkv': b_w_qkv, 'b_lam_q1': b_lam_q1, 'b_lam_k1': b_lam_k1, 'b_lam_q2': b_lam_q2,
            'b_lam_k2': b_lam_k2, 'b_subln_g': b_subln_g, 'b_w_o': b_w_o,
            'ff_w_gate': ff_w_gate, 'ff_w_up': ff_w_up, 'ff_w_down': ff_w_down,
            'moe_w_router': moe_w_router, 'moe_w_gate': moe_w_gate, 'moe_w_up': moe_w_up,
            'moe_w_down': moe_w_down}


def reference(x, c, ctx, c_ctx, w_mod, b_mod, ln_g, ln_b, a_w_qkv, a_sink, a_w_o,
              b_w_qkv, b_lam_q1, b_lam_k1, b_lam_q2, b_lam_k2, b_subln_g, b_w_o,
              ff_w_gate, ff_w_up, ff_w_down, moe_w_router, moe_w_gate, moe_w_up, moe_w_down):
    B, L, D = x.shape
    C = ctx.shape[1]
    cos, sin = axial_rope_tables(L, A_HEAD_DIM, x.dtype)
    xl, xc = x, ctx
    for i in range(DEPTH):
        with_ctx = i < DEPTH - 1
        j = i // 2
        sh_a, sc_a, g_a, sh_f, sc_f, g_f = modulation(c, w_mod[i], b_mod[i])
        csh_a, csc_a, cg_a, csh_f, csc_f, cg_f = modulation(c_ctx, w_mod[i], b_mod[i])

        hl = xl * (1.0 + sc_a[:, None]) + sh_a[:, None]
        hc = xc * (1.0 + csc_a) + csh_a
        if i % 2 == 0:
            ol, oc = windowed_gqa(hl, hc, a_w_qkv[j], a_sink[j], a_w_o[j], cos, sin, with_ctx)
        else:
            lam_init = 0.8 - 0.6 * math.exp(-0.3 * i)
            ol, oc = diff_attention(hl, hc, b_w_qkv[j], b_lam_q1[j], b_lam_k1[j], b_lam_q2[j], b_lam_k2[j],
                                    b_subln_g[j], b_w_o[j], cos, sin, lam_init, with_ctx)
        xl = layer_norm(DEEPNORM_ALPHA * xl + (1.0 + g_a[:, None]) * ol, ln_g[i, 0], ln_b[i, 0])
        if with_ctx:
            xc = layer_norm(DEEPNORM_ALPHA * xc + (1.0 + cg_a) * oc, ln_g[i, 0], ln_b[i, 0])

        hl = xl * (1.0 + sc_f[:, None]) + sh_f[:, None]
        if with_ctx:
            hc = xc * (1.0 + csc_f) + csh_f
            h = jnp.concatenate([hc, hl], axis=1)
        else:
            h = hl
        if i % 2 == 0:
            f = swiglu(h, ff_w_gate[j], ff_w_up[j], ff_w_down[j])
        else:
            f = moe_swiglu(h, moe_w_router[j], moe_w_gate[j], moe_w_up[j], moe_w_down[j])
        if with_ctx:
            fc, fl = f[:, :C], f[:, C:]
            xc = layer_norm(DEEPNORM_ALPHA * xc + (1.0 + cg_f) * fc, ln_g[i, 1], ln_b[i, 1])
        else:
            fl = f
        xl = layer_norm(DEEPNORM_ALPHA * xl + (1.0 + g_f[:, None]) * fl, ln_g[i, 1], ln_b[i, 1])
    return xl
```

```python
import math
from contextlib import ExitStack

import numpy as np
import concourse.bass as bass
import concourse.mybir as mybir
from concourse.bass_utils import run_bass_kernel_spmd

F32 = mybir.dt.float32
BF16 = mybir.dt.bfloat16
AF = mybir.ActivationFunctionType
ALU = mybir.AluOpType
AX = mybir.AxisListType

D = 1024
KC = 8
LAT = 2048
NCTX = 256
TOK = LAT + NCTX
NT = TOK // 128
GROUPS = [(0, 512), (512, 512), (1024, 512), (1536, 512), (2048, 256)]
DEPTH = 4
ALPHA = (2.0 * DEPTH) ** 0.25
LN_EPS = 1e-5
RMS_EPS = 1e-5
D_FF = 2816
D_FFE = 3584
NE = 8
SCALE = 64 ** -0.5
NB = 2


class Buf:
    __slots__ = ("w", "r")

    def __init__(self):
        self.w = {}
        self.r = {}


class FW:
    def __init__(self, nc, es):
        self.nc = nc
        self.es = es
        self.eng = {"pe": nc.tensor, "act": nc.scalar, "dve": nc.vector, "pool": nc.gpsimd, "sp": nc.sync}
        self.sem = {}
        self.cnt = {}
        for k in ("pe", "act", "dve"):
            self.sem[k] = es.enter_context(nc.semaphore("s_" + k))
            self.cnt[k] = 0
        self.seen = {k: {} for k in self.eng}
        self.dsem = {}
        self.nwait = 0
        self.ninst = 0

    def new_dsem(self, name):
        s = self.es.enter_context(self.nc.semaphore("d_" + name))
        self.dsem[name] = [s, 0]

    def _wait(self, e, toks):
        eng = self.eng[e]
        seen = self.seen[e]
        for key, (sem, val) in toks.items():
            if seen.get(key, 0) >= val:
                continue
            eng.wait_ge(sem, val)
            self.nwait += 1
            seen[key] = val

    def _deps(self, e, reads, writes):
        toks = {}
        for b in reads:
            for key, t in b.w.items():
                if toks.get(key, (None, 0))[1] < t[1]:
                    toks[key] = t
        for b in writes:
            for d in (b.w, b.r):
                for key, t in d.items():
                    if key == e:
                        continue
                    if toks.get(key, (None, 0))[1] < t[1]:
                        toks[key] = t
        return toks

    def op(self, e, reads, writes, fn):
        self._wait(e, self._deps(e, reads, writes))
        ins = fn()
        self.cnt[e] += 1
        ins.then_inc(self.sem[e], 1)
        tok = (self.sem[e], self.cnt[e])
        for b in writes:
            b.w[e] = tok
        for b in reads:
            b.r[e] = tok
        return ins

    def fence(self, src, dst):
        for d in dst:
            for s_ in src:
                for dd in (s_.w, s_.r):
                    for key, t in dd.items():
                        if d.r.get(key, (None, 0))[1] < t[1]:
                            d.r[key] = t
                        if d.w.get(key, (None, 0))[1] < t[1]:
                            d.w[key] = t

    def dma(self, q, dsem, reads, writes, out, in_):
        self._wait(q, self._deps(q, reads, writes))
        s = self.dsem[dsem]
        ins = self.eng[q].dma_start(out=out, in_=in_)
        s[1] += 16
        ins.then_inc(s[0], 16)
        tok = (s[0], s[1])
        key = "d_" + dsem
        for b in writes:
            b.w[key] = tok
        for b in reads:
            b.r[key] = tok
        return ins


class Rot:
    def __init__(self, items):
        self.items = items
        self.i = 0

    def next(self):
        it = self.items[self.i % len(self.items)]
        self.i += 1
        return it


def lam_init_of(i):
    return 0.8 - 0.6 * math.exp(-0.3 * i)


def build_nc(NL=DEPTH, nb=NB, STAGE=99):
    nc = bass.Bass("TRN2", target_bir_lowering=False)

    def din(name, shape, dt=F32):
        return nc.dram_tensor(name, list(shape), dt, kind="ExternalInput").ap()

    xT_d = din("xT", [nb, D, TOK])
    cT_d = din("cT", [128, KC * 3])
    wmod_d = din("w_mod", [DEPTH, D, 6 * D])
    bmodT_d = din("bmodT", [128, DEPTH * 48])
    lngT_d = din("lngT", [128, DEPTH * 16])
    lnbT_d = din("lnbT", [128, DEPTH * 16])
    wattn_d = din("wattn", [DEPTH, 8, D, 640])
    wo_d = din("wo", [DEPTH, D, D])
    ffg_d = din("ffg", [2, D, D_FF])
    ffu_d = din("ffu", [2, D, D_FF])
    ffd_d = din("ffd", [2, D_FF, D])
    mog_d = din("mog", [2, NE, D, D_FFE])
    mou_d = din("mou", [2, NE, D, D_FFE])
    mod_d = din("mod", [2, NE, D_FFE, D])
    routT_d = din("routT", [128, 2 * KC * NE])
    cos_d = din("cosT", [128, TOK])
    sin_d = din("sinT", [128, TOK])
    mask_d = din("masks", [128, 512])
    sink_d = din("sink", [1, 32])
    lamv_d = din("lamv", [128, 2 * 4 * 64])
    subl_d = din("sublnT", [128, 2])
    ident_d = din("ident", [128, 128])
    outT_d = nc.dram_tensor("outT", [nb, D, LAT], F32, kind="ExternalOutput").ap()
    ascr_d = nc.dram_tensor("a_scr", [8, 128, TOK], BF16, kind=("ExternalOutput" if STAGE < 99 else "Internal")).ap()

    es = ExitStack()
    with es:
        fw = FW(nc, es)
        op = fw.op

        def sb(name, shape, dt):
            return es.enter_context(nc.sbuf_tensor(name, list(shape), dt))

        xT = sb("xT_sb", [128, KC, TOK], F32)
        bx = [Buf() for _ in GROUPS]
        arena = sb("arena", [128, KC * TOK], BF16)
        bh = [Buf() for _ in GROUPS]
        bset = [Buf() for _ in range(2)]
        bset_a = [Buf() for _ in range(2)]

        def hT(kc, t0, n):
            return arena[:, kc * TOK + t0: kc * TOK + t0 + n]

        def setv(s, which):
            base = s * 4 * TOK + {"q": 0, "k": 1, "v": 2, "a": 3}[which] * TOK
            return arena[:, base: base + TOK]

        NSLOT = 8
        wsl = [sb(f"wsl{i}", [128, 2048], BF16) for i in range(NSLOT)]
        bws = [Buf() for _ in range(NSLOT)]
        wctr = [0]
        hb = [sb(f"hb{i}", [128, KC, 512], BF16) for i in range(2)]
        bhb = [Buf() for _ in range(2)]
        rot_hb = Rot(list(zip(hb, bhb)))
        gbc = sb("gbc", [128, TOK], BF16)
        bgbc = Buf()
        sq = [sb(f"sq{i}", [128, 512], F32) for i in range(2)]
        bsq = [Buf() for _ in range(2)]
        rot_sq = Rot(list(zip(sq, bsq)))
        mean_sb = sb("mean_sb", [128, 512], F32); bmean = Buf()
        rstd_sb = sb("rstd_sb", [128, 512], F32); brstd = Buf()
        lt = [sb(f"lt{i}", [128, 512], F32) for i in range(2)]
        blt = [Buf() for _ in range(2)]
        rot_lt = Rot(list(zip(lt, blt)))
        PT = [sb(f"PT{i}", [128, 512], BF16) for i in range(4)]
        bPT = [Buf() for _ in range(4)]
        rot_PT = Rot(list(zip(PT, bPT)))
        nt_ = [sb(f"ntmp{i}", [128, 512], F32) for i in range(3)]
        bnt = [Buf() for _ in range(3)]
        rot_nt = Rot(list(zip(nt_, bnt)))
        st_bf = [sb(f"stbf{i}", [128, 512], F32) for i in range(2)]
        bst = [Buf() for _ in range(2)]
        rot_st = Rot(list(zip(st_bf, bst)))
        cosb = sb("cosb", [128, TOK], BF16); bcos = Buf()
        sinb = sb("sinb", [128, TOK], BF16); bsin = Buf()
        maskb = sb("maskb", [128, 512], BF16); bmask = Buf()
        ident = sb("ident_sb", [128, 128], F32); bident = Buf()
        onesM = sb("onesM", [128, 128], F32); bones = Buf()
        ones128 = sb("ones128", [128, 128], F32)
        onesb = sb("onesb", [128, 128], BF16)
        sinkL = sb("sinkL", [1, 128], BF16)
        sinkv = sb("sinkv", [1, 32], F32); bsinkv = Buf()
        sinkrow = sb("sinkrow", [1, 256], BF16); bsinkrow = Buf()
        lamv = nt_[0]; blamv = bnt[0]
        lamt = sb("lamt", [128, 16], F32); blam = Buf()
        lamscr = sb("lamscr", [128, 64], F32)
        subl = sb("subl_sb", [128, 2], F32); bsubl = Buf()
        cT = sb("cT_sb", [128, KC * 3], F32); bcT = Buf()
        bmodT = sb("bmodT_sb", [128, DEPTH * 48], F32); bbmod = Buf()
        lngT = sb("lngT_sb", [128, DEPTH * 16], F32); blng = Buf()
        lnbT = sb("lnbT_sb", [128, DEPTH * 16], F32); blnb = Buf()
        routT = sb("routT_sb", [128, 2 * KC * NE], F32); brout = Buf()
        modT = sb("modT", [128, DEPTH * 48 * 3], F32); bmod = Buf()
        lg = sb("lg", [128, 32], F32); blg = Buf()
        rt = sb("rt", [128, 64], F32); brt = Buf()
        gates = sb("gates", [128, NT * 8], F32); bgates = Buf()
        gbt = sb("gbt", [128, 128], F32); bgbt = Buf()
        h32 = lt
        bh32 = blt
        rot_h32 = Rot(list(zip(h32, bh32)))

        ps = [es.enter_context(nc.psum_tensor(f"ps{i}", [128, 512], F32)) for i in range(8)]
        bps = [Buf() for _ in range(8)]
        rotS = Rot(list(zip(ps[0:4], bps[0:4])))
        rotA = Rot(list(zip(ps[4:8], bps[4:8])))

        for n in ["const", "x", "ascr_w", "out", "ar0", "ar1", "wm0", "wm1", "wm2", "wm3"] + [f"w{k}" for k in range(NSLOT)]:
            fw.new_dsem(n)
        rot_hb = Rot([(hb[0], bhb[0], 0), (hb[1], bhb[1], 1)])
        rot_wm = Rot([(h32[0], bh32[0], "wm0"), (h32[1], bh32[1], "wm1"), (sq[0], bsq[0], "wm2"), (sq[1], bsq[1], "wm3")])
        mrow = mean_sb
        bmrow = bmean
        b_ascr = Buf()

        def modcol(i, j, kc, r):
            c = ((i * 48) + j * 8 + kc) * 3 + r
            return modT[:, c: c + 1]

        cb = []

        def cload(q, dst, src, b):
            fw.dma(q, "const", [], [b], dst, src)
            cb.append(b)

        cload("sp", cT[:], cT_d, bcT)
        cload("sp", bmodT[:], bmodT_d, bbmod)
        cload("sp", lngT[:], lngT_d, blng)
        cload("sp", lnbT[:], lnbT_d, blnb)
        cload("sp", routT[:], routT_d, brout)
        cload("sp", ident[:], ident_d, bident)
        cload("sp", sinkv[:], sink_d, bsinkv)
        cload("sp", lamv[:], lamv_d, blamv)
        cload("sp", subl[:], subl_d, bsubl)
        cload("pool", cosb[:], cos_d, bcos)
        cload("pool", sinb[:], sin_d, bsin)
        cload("pool", maskb[:], mask_d, bmask)
        tokc = ("d_const", (fw.dsem["const"][0], fw.dsem["const"][1]))
        for b in cb:
            b.w = {tokc[0]: tokc[1]}

        epsc = sb("epsc", [128, 2], F32)

        def _consts():
            nc.vector.memset(epsc[:, 0:1], LN_EPS / ALPHA ** 2)
            nc.vector.memset(epsc[:, 1:2], RMS_EPS)
            nc.vector.memset(onesM[:], 1.0 / 1024.0)
            nc.vector.memset(ones128[:], 1.0 / 128.0)
            nc.vector.memset(onesb[:], 1.0)
            nc.vector.memset(sinkL[:, 0:64], 0.0)
            return nc.vector.memset(sinkL[:, 64:128], 1.0)
        op("dve", [], [bones], _consts)
        op("act", [bsinkv], [bsinkv], lambda: nc.scalar.activation(sinkv[:], sinkv[:], AF.Exp))

        blscr = Buf()
        for j in range(2):
            for t in range(2):
                a = lamv[:, (j * 4 + 2 * t) * 64:(j * 4 + 2 * t + 1) * 64]
                b_ = lamv[:, (j * 4 + 2 * t + 1) * 64:(j * 4 + 2 * t + 2) * 64]
                op("dve", [blamv], [blscr], lambda: nc.vector.tensor_tensor(lamscr[:], a, b_, ALU.mult))
                op("dve", [blscr], [blam],
                   lambda: nc.vector.reduce_sum(lamt[:, j * 4 + t: j * 4 + t + 1], lamscr[:], AX.X))
        for j in range(2):
            op("act", [blam], [blam],
               lambda: nc.scalar.activation(lamt[:, j * 4: j * 4 + 2], lamt[:, j * 4: j * 4 + 2], AF.Exp))
        for j in range(2):
            li = lam_init_of(2 * j + 1)
            op("dve", [blam], [blam],
               lambda: nc.vector.tensor_tensor(lamt[:, j * 4 + 2: j * 4 + 3], lamt[:, j * 4: j * 4 + 1],
                                               lamt[:, j * 4 + 1: j * 4 + 2], ALU.subtract))
            op("dve", [blam], [blam],
               lambda: nc.vector.tensor_scalar(lamt[:, j * 4 + 3: j * 4 + 4], lamt[:, j * 4 + 2: j * 4 + 3],
                                               li, -1.0, op0=ALU.add, op1=ALU.mult))
            op("dve", [bsubl], [bsubl],
               lambda: nc.vector.tensor_scalar(subl[:, j:j + 1], subl[:, j:j + 1], 1.0 - li, None, op0=ALU.mult))

        op("act", [bcT], [bcT], lambda: nc.scalar.activation(cT[:], cT[:], AF.Silu))
        for i in range(NL):
            wv = wmod_d[i].rearrange("(kc p) n -> p kc n", p=128)
            for ncx in range(12):
                pb, bpb = rotA.next()
                for kc in range(KC):
                    t, bt, dn = rot_wm.next()
                    fw.dma("sp", dn, [], [bt], t[:], wv[:, kc, ncx * 512:(ncx + 1) * 512])
                    op("pe", [bcT, bt], [bpb],
                       lambda: nc.tensor.matmul(pb[0:3, :], cT[:, kc * 3:(kc + 1) * 3], t[:],
                                                start=(kc == 0), stop=(kc == KC - 1)))
                op("act", [bpb], [bmrow], lambda: nc.scalar.copy(mrow[0:3, :], pb[0:3, :]))
                tb, btb = rotS.next()

                def _tr():
                    ins = None
                    for q in range(4):
                        ins = nc.tensor.transpose(tb[:, q * 3:(q + 1) * 3], mrow[0:3, q * 128:(q + 1) * 128],
                                                  ident[0:3, 0:3])
                    return ins
                op("pe", [bmrow, bident], [btb], _tr)

                def _ev():
                    ins = None
                    for q in range(4):
                        oc = ncx * 4 + q
                        c = (i * 48 + oc) * 3
                        ins = nc.vector.tensor_scalar(modT[:, c:c + 3], tb[:, q * 3:(q + 1) * 3],
                                                      bmodT[:, i * 48 + oc:i * 48 + oc + 1], None, op0=ALU.add)
                    return ins
                op("dve", [btb, bbmod], [bmod], _ev)

            def _der():
                ins = None
                for j in (1, 4):
                    c = (i * 48 + j * 8) * 3
                    ins = nc.vector.tensor_scalar(modT[:, c:c + 24], modT[:, c:c + 24], 1.0, None, op0=ALU.add)
                for j in (2, 5):
                    c = (i * 48 + j * 8) * 3
                    ins = nc.vector.tensor_scalar(modT[:, c:c + 24], modT[:, c:c + 24], 1.0, 1.0 / ALPHA,
                                                  op0=ALU.add, op1=ALU.mult)
                return ins
            op("dve", [bmod], [bmod], _der)

        def wload(src_ap, a, bb):
            k = wctr[0] % NSLOT
            wctr[0] += 1
            view = wsl[k][:, 0:a * bb].rearrange("p (a b) -> p a b", a=a)
            fw.dma("pool", f"w{k}", [], [bws[k]], view, src_ap)
            return view, bws[k]

        rot_rstd = Rot([(rstd_sb, brstd), (mean_sb, bmean)])

        def ln_stats1(i, s_, g):
            t0, n = GROUPS[g]
            mp = rotA.next()
            qp = rotA.next()
            for kc in range(KC):
                sqt = rot_sq.next()
                op("act", [bx[g]], [sqt[1]],
                   lambda: nc.scalar.activation(sqt[0][:, 0:n], xT[:, kc, t0:t0 + n], AF.Square))

                def _mm():
                    nc.tensor.matmul(mp[0][:, 0:n], onesM[:], xT[:, kc, t0:t0 + n], start=(kc == 0), stop=(kc == KC - 1))
                    return nc.tensor.matmul(qp[0][:, 0:n], onesM[:], sqt[0][:, 0:n], start=(kc == 0), stop=(kc == KC - 1))
                op("pe", [bx[g], sqt[1], bones], [mp[1], qp[1]], _mm)
            return (mp, qp)

        def ln_stats2(i, s_, g, st):
            t0, n = GROUPS[g]
            mp, qp = st
            m2 = rot_lt.next()
            op("act", [mp[1]], [m2[1]], lambda: nc.scalar.activation(m2[0][:, 0:n], mp[0][:, 0:n], AF.Square))
            op("dve", [qp[1], m2[1]], [m2[1]],
               lambda: nc.vector.tensor_tensor(m2[0][:, 0:n], qp[0][:, 0:n], m2[0][:, 0:n], ALU.subtract))
            rs = rot_rstd.next()
            op("act", [m2[1], bones], [rs[1]],
               lambda: nc.scalar.activation(rs[0][:, 0:n], m2[0][:, 0:n], AF.Ln, bias=epsc[:, 0:1], scale=1.0))
            op("act", [rs[1]], [rs[1]],
               lambda: nc.scalar.activation(rs[0][:, 0:n], rs[0][:, 0:n], AF.Exp, scale=-0.5))
            return (mp, rs)

        def ln_apply(i, s_, g, st):
            t0, n = GROUPS[g]
            mp, rs = st
            for kc in range(KC):
                t = rot_lt.next()
                op("dve", [bx[g], mp[1]], [t[1]],
                   lambda: nc.vector.tensor_tensor(t[0][:, 0:n], xT[:, kc, t0:t0 + n], mp[0][:, 0:n], ALU.subtract))
                op("dve", [t[1], rs[1]], [t[1]],
                   lambda: nc.vector.tensor_tensor(t[0][:, 0:n], t[0][:, 0:n], rs[0][:, 0:n], ALU.mult))
                c = (i * 2 + s_) * 8 + kc
                op("act", [t[1], blng, blnb], [bx[g]],
                   lambda: nc.scalar.activation(xT[:, kc, t0:t0 + n], t[0][:, 0:n], AF.Identity,
                                                bias=lnbT[:, c:c + 1], scale=lngT[:, c:c + 1]))

        class LNPipe:
            def __init__(self, i, s_):
                self.i, self.s_, self.prev = i, s_, None

            def push(self, g):
                st1 = ln_stats1(self.i, self.s_, g)
                if self.prev is not None:
                    ln_apply(self.i, self.s_, self.prev[0], self.prev[1])
                st2 = ln_stats2(self.i, self.s_, g, st1)
                self.prev = (g, st2)

            def flush(self):
                if self.prev is not None:
                    ln_apply(self.i, self.s_, self.prev[0], self.prev[1])
                    self.prev = None

        for bi in range(nb):
            def rowof(g):
                return 2 if g == 4 else bi

            for kc in range(KC):
                fw.dma("sp", "x", [], bx, xT[:, kc, :], xT_d[bi, kc * 128:(kc + 1) * 128, :])

            for i in range(NL if STAGE >= 2 else 0):
                with_ctx = i < DEPTH - 1
                isA = (i % 2 == 0)
                j = i // 2
                ngr = 5 if with_ctx else 4
                fw.fence(bh, bset + bset_a)
                wa = wattn_d[i]
                for u in range(8 if STAGE >= 3.7 else 1):
                    s = u % 2
                    qv, kv, vv, av = setv(s, "q"), setv(s, "k"), setv(s, "v"), setv(s, "a")
                    wu_ = wa[u].rearrange("(kc p) n -> p kc n", p=128)
                    wq, bwq = wload(wu_[:, :, 0:256], 8, 256)
                    wk, bwk = wload(wu_[:, :, 256:512], 8, 256)
                    wvv, bwv = wload(wu_[:, :, 512:640], 8, 128)
                    vw = 64 if isA else 128
                    if isA:
                        op("dve", [], [bset[s]],
                           lambda: nc.vector.memset(vv.rearrange("p (t c) -> p t c", c=128)[:, :, 64:128], 1.0))

                        def _sr():
                            nc.vector.tensor_copy(sinkrow[0:1, 0:128],
                                                  sinkv[0:1, j * 16 + 2 * u: j * 16 + 2 * u + 1].to_broadcast([1, 128]))
                            return nc.vector.tensor_copy(sinkrow[0:1, 128:256],
                                                         sinkv[0:1, j * 16 + 2 * u + 1: j * 16 + 2 * u + 2].to_broadcast([1, 128]))
                        op("dve", [bsinkv], [bsinkrow], _sr)
                    def emit_mh(g_):
                        t0_, n_g = GROUPS[g_]
                        r_ = rowof(g_)
                        hb_, bhb_, _ = rot_hb.next()

                        def _mh():
                            ins = None
                            for kc in range(KC):
                                ins = nc.scalar.activation(hb_[:, kc, 0:n_g], xT[:, kc, t0_:t0_ + n_g], AF.Identity,
                                                           bias=modcol(i, 0, kc, r_), scale=modcol(i, 1, kc, r_))
                            return ins
                        op("act", [bx[g_], bmod], [bhb_], _mh)
                        return hb_, bhb_
                    nxt_h = emit_mh(0)
                    for g in range(5):
                        t0, n = GROUPS[g]
                        r = rowof(g)
                        hbt, bhbt = nxt_h
                        for (w, bw, dstv) in ((wq, bwq, qv), (wk, bwk, kv)):
                            p1, bp1 = rotS.next()
                            p2, bp2 = rotS.next()

                            def _mm():
                                ins = None
                                for kc in range(KC):
                                    nc.tensor.matmul(p1[:, 0:n], w[:, kc, 0:128], hbt[:, kc, 0:n],
                                                     start=(kc == 0), stop=(kc == KC - 1))
                                for kc in range(KC):
                                    ins = nc.tensor.matmul(p2[:, 0:n], w[:, kc, 128:256], hbt[:, kc, 0:n],
                                                           start=(kc == 0), stop=(kc == KC - 1))
                                return ins
                            op("pe", [bw, bhbt], [bp1, bp2], _mm)
                            t1, bt1 = rot_nt.next()
                            t2, bt2 = rot_nt.next()
                            op("dve", [bp1, bcos], [bt1],
                               lambda: nc.vector.tensor_tensor(t1[:, 0:n], p1[:, 0:n], cosb[:, t0:t0 + n], ALU.mult))
                            op("dve", [bp2, bsin], [bt2],
                               lambda: nc.vector.tensor_tensor(t2[:, 0:n], p2[:, 0:n], sinb[:, t0:t0 + n], ALU.mult))
                            op("dve", [bt1, bt2], [bset[s]],
                               lambda: nc.vector.tensor_tensor(dstv[:, t0:t0 + n], t1[:, 0:n], t2[:, 0:n], ALU.add))
                        pv, bpv = rotS.next()
                        ntile = n // 128

                        def _mmv():
                            ins = None
                            for tt in range(ntile):
                                for kc in range(KC):
                                    ins = nc.tensor.matmul(pv[:, tt * 128: tt * 128 + vw],
                                                           hbt[:, kc, tt * 128:(tt + 1) * 128], wvv[:, kc, 0:vw],
                                                           start=(kc == 0), stop=(kc == KC - 1))
                            return ins
                        op("pe", [bwv, bhbt], [bpv], _mmv)
                        if g + 1 < 5:
                            nxt_h = emit_mh(g + 1)

                        def _evv():
                            ins = None
                            for tt in range(ntile):
                                o0 = (t0 // 128 + tt) * 128
                                ins = nc.scalar.copy(vv[:, o0:o0 + vw], pv[:, tt * 128: tt * 128 + vw])
                            return ins
                        op("act", [bpv], [bset[s]], _evv)

                    if STAGE < 3:
                        continue
                    if isA:
                        nblocks = 18 if with_ctx else 16
                        items = [(n_, hh) for n_ in range(nblocks) for hh in range(2)]
                        a_state = {}

                        def kts_of(n_):
                            if n_ >= 16:
                                return [(16, None), (17, None)]
                            kts = []
                            if n_ > 0:
                                kts.append((n_ - 1, "prev"))
                            kts.append((n_, None))
                            if n_ < 15:
                                kts.append((n_ + 1, "next"))
                            return kts + [(16, None), (17, None)]

                        def a_stage1(n_, hh):
                            kts = kts_of(n_)
                            nk = len(kts)
                            q0 = n_ * 128
                            pr = slice(64 * hh, 64 * hh + 64)
                            bk = [rotS.next()] + ([rotS.next()] if nk > 4 else [])

                            def _qk():
                                ins = None
                                for idx, (kt, m) in enumerate(kts):
                                    bank = bk[idx // 4][0]
                                    c0 = (idx % 4) * 128
                                    ins = nc.tensor.matmul(bank[:, c0:c0 + 128], kv[pr, kt * 128:(kt + 1) * 128],
                                                           qv[pr, q0:q0 + 128], start=True, stop=True)
                                return ins
                            op("pe", [bset[s]], [b for _, b in bk], _qk)
                            pts = []
                            for bi_, (bank, bbank) in enumerate(bk):
                                width = min(nk - 4 * bi_, 4) * 128
                                pt, bpt = rot_PT.next()
                                op("act", [bbank], [bpt],
                                   lambda: nc.scalar.activation(pt[:, 0:width], bank[:, 0:width], AF.Exp, scale=SCALE))
                                pts.append((pt, bpt))
                            for idx, (kt, m) in enumerate(kts):
                                if m:
                                    pt, bpt = pts[idx // 4]
                                    c0 = (idx % 4) * 128
                                    mo = 0 if m == "prev" else 256
                                    op("dve", [bpt, bmask], [bpt],
                                       lambda: nc.vector.tensor_tensor(pt[:, c0:c0 + 128], pt[:, c0:c0 + 128],
                                                                       maskb[:, mo:mo + 128], ALU.mult))
                            a_state[(n_, hh)] = (kts, pts)

                        def a_stage2(n_, hh):
                            kts, pts = a_state.pop((n_, hh))
                            q0 = n_ * 128
                            acc, bacc = rotA.next()

                            def _pv():
                                for idx, (kt, m) in enumerate(kts):
                                    pt = pts[idx // 4][0]
                                    c0 = (idx % 4) * 128
                                    nc.tensor.matmul(acc[:, 0:128], vv[:, kt * 128:(kt + 1) * 128], pt[:, c0:c0 + 128],
                                                     start=(idx == 0), stop=False)
                                return nc.tensor.matmul(acc[:, 0:128], sinkL[0:1, :], sinkrow[0:1, hh * 128:(hh + 1) * 128],
                                                        start=False, stop=True)
                            op("pe", [bset[s], bsinkrow, bones] + [b for _, b in pts], [bacc], _pv)
                            rc, brc = rot_nt.next()
                            op("act", [bacc], [brc],
                               lambda: nc.scalar.activation(rc[0:64, 0:128], acc[64:128, 0:128], AF.Ln))
                            op("act", [brc], [brc],
                               lambda: nc.scalar.activation(rc[0:64, 0:128], rc[0:64, 0:128], AF.Exp, scale=-1.0))
                            op("dve", [bacc, brc], [bset_a[s]],
                               lambda: nc.vector.tensor_tensor(av[64 * hh:64 * hh + 64, q0:q0 + 128], acc[0:64, 0:128],
                                                               rc[0:64, 0:128], ALU.mult))
                        a_stage1(*items[0])
                        for k_, it in enumerate(items):
                            if k_ + 1 < len(items):
                                a_stage1(*items[k_ + 1])
                            a_stage2(*it)
                    else:
                        o1, o2, d1, d2 = ps[4], ps[5], ps[6], ps[7]
                        accb = [bps[4], bps[5], bps[6], bps[7]]
                        pend2 = [None]
                        for g in range(ngr):
                            t0, n = GROUPS[g]
                            kts = list(range(NT)) if g < 4 else [16, 17]
                            Sb = {}

                            def qk(kt):
                                s1 = rotS.next()
                                s2 = rotS.next()

                                def _f():
                                    nc.tensor.matmul(s1[0][:, 0:n], kv[0:64, kt * 128:(kt + 1) * 128],
                                                     qv[0:64, t0:t0 + n], start=True, stop=True)
                                    return nc.tensor.matmul(s2[0][:, 0:n], kv[64:128, kt * 128:(kt + 1) * 128],
                                                            qv[64:128, t0:t0 + n], start=True, stop=True)
                                op("pe", [bset[s]], [s1[1], s2[1]], _f)
                                p1 = rot_PT.next()
                                p2 = rot_PT.next()
                                op("act", [s1[1]], [p1[1]],
                                   lambda: nc.scalar.activation(p1[0][:, 0:n], s1[0][:, 0:n], AF.Exp, scale=SCALE))
                                op("act", [s2[1]], [p2[1]],
                                   lambda: nc.scalar.activation(p2[0][:, 0:n], s2[0][:, 0:n], AF.Exp, scale=SCALE))
                                Sb[kt] = (p1, p2)
                            qk(kts[0])
                            for idx, kt in enumerate(kts):
                                if idx + 1 < len(kts):
                                    qk(kts[idx + 1])
                                p1, p2 = Sb.pop(kt)
                                st_ = (idx == 0)
                                sp_ = (idx == len(kts) - 1)

                                def _pv():
                                    nc.tensor.matmul(o1[:, 0:n], vv[:, kt * 128:(kt + 1) * 128], p1[0][:, 0:n], start=st_, stop=sp_)
                                    nc.tensor.matmul(d1[:, 0:n], onesb[:], p1[0][:, 0:n], start=st_, stop=sp_)
                                    nc.tensor.matmul(o2[:, 0:n], vv[:, kt * 128:(kt + 1) * 128], p2[0][:, 0:n], start=st_, stop=sp_)
                                    return nc.tensor.matmul(d2[:, 0:n], onesb[:], p2[0][:, 0:n], start=st_, stop=sp_)
                                op("pe", [bset[s], p1[1], p2[1], bones], accb, _pv)
                                if idx == 1 and pend2[0] is not None:
                                    pend2[0]()
                                    pend2[0] = None
                            if pend2[0] is not None:
                                pend2[0]()
                                pend2[0] = None
                            r1 = rot_nt.next()
                            r2 = rot_nt.next()
                            o = rot_nt.next()
                            op("act", [bps[6]], [r1[1]], lambda: nc.scalar.activation(r1[0][:, 0:n], d1[:, 0:n], AF.Ln))
                            op("act", [bps[7]], [r2[1]], lambda: nc.scalar.activation(r2[0][:, 0:n], d2[:, 0:n], AF.Ln))
                            op("act", [r1[1]], [r1[1]],
                               lambda: nc.scalar.activation(r1[0][:, 0:n], r1[0][:, 0:n], AF.Exp, scale=-1.0))
                            op("act", [r2[1]], [r2[1]],
                               lambda: nc.scalar.activation(r2[0][:, 0:n], r2[0][:, 0:n], AF.Exp, scale=-1.0))
                            op("dve", [bps[4], r1[1]], [r1[1]],
                               lambda: nc.vector.tensor_tensor(r1[0][:, 0:n], o1[:, 0:n], r1[0][:, 0:n], ALU.mult))
                            op("dve", [bps[5], r2[1]], [r2[1]],
                               lambda: nc.vector.tensor_tensor(r2[0][:, 0:n], o2[:, 0:n], r2[0][:, 0:n], ALU.mult))
                            def _part2(r1=r1, r2=r2, o=o, n=n, t0=t0):
                              op("dve", [r1[1], r2[1], blam], [o[1]],
                                 lambda: nc.vector.scalar_tensor_tensor(o[0][:, 0:n], r2[0][:, 0:n],
                                                                        lamt[:, j * 4 + 3: j * 4 + 4], r1[0][:, 0:n],
                                                                        op0=ALU.mult, op1=ALU.add))
                              sqt = rot_sq.next()
                              op("act", [o[1]], [sqt[1]],
                                 lambda: nc.scalar.activation(sqt[0][:, 0:n], o[0][:, 0:n], AF.Square))
                              mp = rotS.next()
                              op("pe", [sqt[1], bones], [mp[1]],
                                 lambda: nc.tensor.matmul(mp[0][:, 0:n], ones128[:], sqt[0][:, 0:n], start=True, stop=True))
                              rs = rot_lt.next()
                              op("act", [mp[1], bones], [rs[1]],
                                 lambda: nc.scalar.activation(rs[0][:, 0:n], mp[0][:, 0:n], AF.Ln, bias=epsc[:, 1:2], scale=1.0))
                              op("act", [rs[1]], [rs[1]],
                                 lambda: nc.scalar.activation(rs[0][:, 0:n], rs[0][:, 0:n], AF.Exp, scale=-0.5))
                              op("dve", [o[1], rs[1]], [o[1]],
                                 lambda: nc.vector.tensor_tensor(o[0][:, 0:n], o[0][:, 0:n], rs[0][:, 0:n], ALU.mult))
                              op("act", [o[1], bsubl], [bset_a[s]],
                                 lambda: nc.scalar.activation(av[:, t0:t0 + n], o[0][:, 0:n], AF.Identity,
                                                              scale=subl[:, j:j + 1]))
                            pend2[0] = _part2
                        if pend2[0] is not None:
                            pend2[0]()
                            pend2[0] = None
                    if STAGE >= 3.15:
                        fw.dma("sp", "ascr_w", [bset_a[s]], [b_ascr], ascr_d[u], av)

                if STAGE < 4:
                    continue
                wov = wo_d[i].rearrange("(kc p) n -> p kc n", p=128)
                asv = ascr_d.rearrange("u p t -> p u t")
                lnp = LNPipe(i, 0)
                for g in range(ngr):
                    t0, n = GROUPS[g]
                    r = rowof(g)
                    hbt, bhbt, hk = rot_hb.next()
                    fw.dma("sp", f"ar{hk}", [b_ascr], [bhbt], hbt[:, :, 0:n], asv[:, :, t0:t0 + n])
                    pcs = [wload(wov[:, 2 * q:2 * q + 2, :], 2, 1024) for q in range(4)]
                    for oc in range(KC):
                        bank = rotS.next()

                        def _mm():
                            ins = None
                            for kc in range(KC):
                                ins = nc.tensor.matmul(bank[0][:, 0:n], pcs[kc // 2][0][:, kc % 2, oc * 128:(oc + 1) * 128],
                                                       hbt[:, kc, 0:n], start=(kc == 0), stop=(kc == KC - 1))
                            return ins
                        op("pe", [bhbt] + [p[1] for p in pcs], [bank[1]], _mm)
                        op("dve", [bank[1], bx[g], bmod], [bx[g]],
                           lambda: nc.vector.scalar_tensor_tensor(xT[:, oc, t0:t0 + n], bank[0][:, 0:n],
                                                                  modcol(i, 2, oc, r), xT[:, oc, t0:t0 + n],
                                                                  op0=ALU.mult, op1=ALU.add))
                    lnp.push(g)
                lnp.flush()

                if STAGE < 5 and i == NL - 1:
                    continue
                moe = not isA
                fw.fence(bset + bset_a, bh)
                for g in range(ngr):
                    t0, n = GROUPS[g]
                    r = rowof(g)
                    ntile = n // 128
                    lgp = rotA.next() if moe else None
                    for kc in range(KC):
                        h = rot_h32.next()
                        op("dve", [bx[g], bmod], [h[1]],
                           lambda: nc.vector.tensor_scalar(h[0][:, 0:n], xT[:, kc, t0:t0 + n],
                                                           modcol(i, 4, kc, r), modcol(i, 3, kc, r),
                                                           op0=ALU.mult, op1=ALU.add))
                        op("act", [h[1]], [bh[g]], lambda: nc.scalar.copy(hT(kc, t0, n), h[0][:, 0:n]))
                        if moe:
                            def _r():
                                ins = None
                                for tt in range(ntile):
                                    ins = nc.tensor.matmul(lgp[0][:, tt * 8:(tt + 1) * 8], h[0][:, tt * 128:(tt + 1) * 128],
                                                           routT[:, (j * 8 + kc) * 8:(j * 8 + kc + 1) * 8],
                                                           start=(kc == 0 and tt == 0), stop=(kc == KC - 1))
                                return ins
                            op("pe", [h[1], brout], [lgp[1]], _r)
                    if moe:
                        op("act", [lgp[1]], [blg], lambda: nc.scalar.copy(lg[:, 0:ntile * 8], lgp[0][:, 0:ntile * 8]))
                        for tt in range(ntile):
                            L = lg[:, tt * 8:(tt + 1) * 8]
                            gi = (t0 // 128 + tt) * 8
                            op("dve", [blg], [brt], lambda: nc.vector.max(out=rt[:, 0:8], in_=L))

                            def _b():
                                nc.vector.tensor_scalar(rt[:, 8:9], rt[:, 0:1], -1.0, None, op0=ALU.mult)
                                return nc.vector.tensor_scalar(rt[:, 16:24], L, rt[:, 1:2], None, op0=ALU.is_ge)
                            op("dve", [blg, brt], [brt], _b)
                            op("act", [blg, brt], [brt],
                               lambda: nc.scalar.activation(rt[:, 24:32], L, AF.Exp, bias=rt[:, 8:9], scale=1.0))
                            op("dve", [brt], [brt],
                               lambda: nc.vector.tensor_tensor(rt[:, 32:40], rt[:, 24:32], rt[:, 16:24], ALU.mult))
                            op("dve", [brt], [brt], lambda: nc.vector.reduce_sum(rt[:, 40:41], rt[:, 32:40], AX.X))
                            op("dve", [brt], [brt], lambda: nc.vector.reciprocal(rt[:, 41:42], rt[:, 40:41]))
                            op("dve", [brt], [bgates],
                               lambda: nc.vector.tensor_scalar(gates[:, gi:gi + 8], rt[:, 32:40], rt[:, 41:42], None,
                                                               op0=ALU.mult))
                if moe:
                    experts = list(range(NE))
                    nchunks = D_FFE // 128
                else:
                    experts = [None]
                    nchunks = D_FF // 128
                units = [(c, min(4, nchunks - c)) for c in range(0, nchunks, 4)]
                glist = list(range(ngr))
                for e in experts:
                    if moe:
                        Wg, Wu, Wd = mog_d[j, e], mou_d[j, e], mod_d[j, e]
                        for g in glist:
                            t0, n = GROUPS[g]
                            bank = rotS.next()
                            for tt in range(n // 128):
                                gi = (t0 // 128 + tt) * 8 + e
                                op("dve", [bgates], [bgbt],
                                   lambda: nc.vector.tensor_copy(gbt[:], gates[:, gi:gi + 1].to_broadcast([128, 128])))
                                op("pe", [bgbt, bident], [bank[1]],
                                   lambda: nc.tensor.matmul(bank[0][:, tt * 128:(tt + 1) * 128], gbt[:], ident[:],
                                                            start=True, stop=True))
                            op("act", [bank[1]], [bgbc], lambda: nc.scalar.copy(gbc[:, t0:t0 + n], bank[0][:, 0:n]))
                    else:
                        Wg, Wu, Wd = ffg_d[j], ffu_d[j], ffd_d[j]
                    Wgv = Wg.rearrange("(kc p) n -> p kc n", p=128)
                    Wuv = Wu.rearrange("(kc p) n -> p kc n", p=128)
                    Wdv = Wd.rearrange("(c p) n -> p c n", p=128)
                    for (c0, nch) in units:
                        halves = (nch + 1) // 2
                        wg = [wload(Wgv[:, :, (c0 + 2 * h_) * 128:(c0 + 2 * h_ + 2) * 128], 8, 256) for h_ in range(halves)]
                        wu = [wload(Wuv[:, :, (c0 + 2 * h_) * 128:(c0 + 2 * h_ + 2) * 128], 8, 256) for h_ in range(halves)]
                        wd = [wload(Wdv[:, c0 + 2 * h_: c0 + 2 * h_ + 2, :], 2, 1024) for h_ in range(halves)]
                        pending = None
                        for g in glist + [None]:
                            actb = None
                            if g is not None:
                                t0, n = GROUPS[g]
                                actb = rot_hb.next()
                                for c in range(nch):
                                    gb = rotS.next()
                                    ub = rotS.next()

                                    def _mm():
                                        ins = None
                                        for kc in range(KC):
                                            nc.tensor.matmul(gb[0][:, 0:n], wg[c // 2][0][:, kc, (c % 2) * 128:(c % 2 + 1) * 128],
                                                             hT(kc, t0, n), start=(kc == 0), stop=(kc == KC - 1))
                                        for kc in range(KC):
                                            ins = nc.tensor.matmul(ub[0][:, 0:n], wu[c // 2][0][:, kc, (c % 2) * 128:(c % 2 + 1) * 128],
                                                                   hT(kc, t0, n), start=(kc == 0), stop=(kc == KC - 1))
                                        return ins
                                    op("pe", [bh[g], wg[c // 2][1], wu[c // 2][1]], [gb[1], ub[1]], _mm)
                                    st = rot_st.next()
                                    op("act", [gb[1]], [st[1]],
                                       lambda: nc.scalar.activation(st[0][:, 0:n], gb[0][:, 0:n], AF.Silu))
                                    if moe:
                                        op("dve", [st[1], ub[1]], [st[1]],
                                           lambda: nc.vector.tensor_tensor(st[0][:, 0:n], st[0][:, 0:n], ub[0][:, 0:n], ALU.mult))
                                        op("dve", [st[1], bgbc], [actb[1]],
                                           lambda: nc.vector.tensor_tensor(actb[0][:, c, 0:n], st[0][:, 0:n],
                                                                           gbc[:, t0:t0 + n], ALU.mult))
                                    else:
                                        op("dve", [st[1], ub[1]], [actb[1]],
                                           lambda: nc.vector.tensor_tensor(actb[0][:, c, 0:n], st[0][:, 0:n],
                                                                           ub[0][:, 0:n], ALU.mult))
                            if pending is not None:
                                pg, pact = pending
                                pt0, pn = GROUPS[pg]
                                pr = rowof(pg)
                                for oc in range(KC):
                                    bank = rotA.next()

                                    def _dn():
                                        ins = None
                                        for c in range(nch):
                                            ins = nc.tensor.matmul(bank[0][:, 0:pn],
                                                                   wd[c // 2][0][:, c % 2, oc * 128:(oc + 1) * 128],
                                                                   pact[0][:, c, 0:pn], start=(c == 0), stop=(c == nch - 1))
                                        return ins
                                    op("pe", [pact[1]] + [w_[1] for w_ in wd], [bank[1]], _dn)
                                    op("dve", [bank[1], bx[pg], bmod], [bx[pg]],
                                       lambda: nc.vector.scalar_tensor_tensor(xT[:, oc, pt0:pt0 + pn], bank[0][:, 0:pn],
                                                                              modcol(i, 5, oc, pr), xT[:, oc, pt0:pt0 + pn],
                                                                              op0=ALU.mult, op1=ALU.add))
                            pending = (g, actb) if g is not None else None
                lnp = LNPipe(i, 1)
                for g in glist:
                    lnp.push(g)
                lnp.flush()

            ov = outT_d[bi].rearrange("(kc p) t -> p kc t", p=128)
            for g in range(4):
                t0, n = GROUPS[g]
                fw.dma("sp", "out", [bx[g]], [], ov[:, :, t0:t0 + n], xT[:, :, t0:t0 + n])
        so = fw.dsem["out"]
        nc.sync.wait_ge(so[0], so[1])
        print("build: cnt", fw.cnt, "waits", fw.nwait)
    return nc


def _rope_tables():
    half = 32
    inv = 10000.0 ** (-np.arange(0, half, 2, dtype=np.float32) / half)
    t = np.arange(LAT)
    row = (t // 64).astype(np.float32)
    col = (t % 64).astype(np.float32)
    ang_r = row[:, None] * inv[None, :]
    ang_c = col[:, None] * inv[None, :]
    ang = np.concatenate([ang_r, ang_r, ang_c, ang_c], axis=-1).astype(np.float32)
    cos = np.cos(ang).astype(np.float32)
    sin = np.sin(ang).astype(np.float32)
    sign = np.concatenate([-np.ones(16), np.ones(16), -np.ones(16), np.ones(16)]).astype(np.float32)
    cosT = np.ones((128, TOK), np.float32)
    sinT = np.zeros((128, TOK), np.float32)
    cosT[0:64, 0:LAT] = cos.T
    cosT[64:128, 0:LAT] = cos.T
    sinT[0:64, 0:LAT] = (sin * sign[None, :]).T
    sinT[64:128, 0:LAT] = (sin * sign[None, :]).T
    return cosT, sinT


def _perm128():
    p64 = np.concatenate([np.arange(16, 32), np.arange(0, 16), np.arange(48, 64), np.arange(32, 48)])
    return np.concatenate([p64, 64 + p64])


def _colT(v):
    v = np.asarray(v, np.float32)
    lead = v.shape[:-1]
    n = v.shape[-1] // 128
    a = v.reshape(lead + (n, 128))
    a = np.moveaxis(a, -1, 0)
    return np.ascontiguousarray(a.reshape(128, -1))


def prepare_shared(inp):
    f = lambda k: np.asarray(inp[k], np.float32)
    perm = _perm128()
    a_w = f("a_w_qkv")
    b_w = f("b_w_qkv")
    wattn = np.zeros((DEPTH, 8, D, 640), np.float32)
    for i in range(DEPTH):
        j = i // 2
        for u in range(8):
            if i % 2 == 0:
                W = a_w[j]
                q = W[:, 128 * u:128 * u + 128]
                g = u // 2
                kc = W[:, 1024 + 64 * g: 1024 + 64 * g + 64]
                k = np.concatenate([kc, kc], axis=1)
                v = np.zeros((D, 128), np.float32)
                v[:, 0:64] = W[:, 1280 + 64 * g: 1280 + 64 * g + 64]
            else:
                W = b_w[j]
                q = W[:, 128 * u:128 * u + 128]
                k = W[:, 1024 + 128 * u: 1024 + 128 * u + 128]
                v = W[:, 2048 + 128 * u: 2048 + 128 * u + 128]
            wattn[i, u, :, 0:128] = q
            wattn[i, u, :, 128:256] = q[:, perm]
            wattn[i, u, :, 256:384] = k
            wattn[i, u, :, 384:512] = k[:, perm]
            wattn[i, u, :, 512:640] = v
    wo = np.zeros((DEPTH, D, D), np.float32)
    a_o = f("a_w_o")
    b_o = f("b_w_o")
    for i in range(DEPTH):
        wo[i] = a_o[i // 2] if i % 2 == 0 else b_o[i // 2]
    cosT, sinT = _rope_tables()
    jj = np.arange(128)[:, None]
    ii = np.arange(128)[None, :]
    mprev = (jj >= ii).astype(np.float32)
    mnext = (jj <= ii).astype(np.float32)
    masks = np.concatenate([mprev, mprev, mnext, mnext], axis=1)
    lam = np.stack([np.stack([f("b_lam_q1")[j], f("b_lam_k1")[j], f("b_lam_q2")[j], f("b_lam_k2")[j]]) for j in range(2)])
    lamv = np.ascontiguousarray(np.broadcast_to(lam.reshape(1, -1), (128, 512))).astype(np.float32)
    rout = f("moe_w_router")
    routT = np.ascontiguousarray(rout.reshape(2, KC, 128, NE).transpose(2, 0, 1, 3).reshape(128, -1))
    shared = {
        "w_mod": f("w_mod"),
        "bmodT": _colT(f("b_mod")),
        "lngT": _colT(f("ln_g")),
        "lnbT": _colT(f("ln_b")),
        "wattn": wattn,
        "wo": wo,
        "ffg": f("ff_w_gate"), "ffu": f("ff_w_up"), "ffd": f("ff_w_down"),
        "mog": f("moe_w_gate"), "mou": f("moe_w_up"), "mod": f("moe_w_down"),
        "routT": routT,
        "cosT": cosT, "sinT": sinT, "masks": masks,
        "sink": np.ascontiguousarray(f("a_sink").reshape(1, 32)),
        "lamv": lamv,
        "sublnT": np.ascontiguousarray(f("b_subln_g").T),
        "ident": np.eye(128, dtype=np.float32),
    }
    return shared


def prepare_core(inp, batches):
    x = np.asarray(inp["x"], np.float32)
    ctx = np.asarray(inp["ctx"], np.float32)
    c = np.asarray(inp["c"], np.float32)
    c_ctx = np.asarray(inp["c_ctx"], np.float32)
    xT = np.empty((len(batches), D, TOK), np.float32)
    for k, b in enumerate(batches):
        xT[k, :, 0:LAT] = x[b].T
        xT[k, :, LAT:] = ctx[b].T
    rows = [c[b] for b in batches]
    while len(rows) < 2:
        rows.append(c[batches[0]])
    rows.append(c_ctx)
    rows = np.stack(rows)
    cT = np.ascontiguousarray(rows.reshape(3, KC, 128).transpose(2, 1, 0).reshape(128, KC * 3))
    return {"xT": xT, "cT": cT}


_NC_CACHE = {}


def kernel(**inputs):
    ncores = 8
    shared = prepare_shared(inputs)
    in_maps = []
    for cidx in range(ncores):
        m = dict(shared)
        m.update(prepare_core(inputs, [NB * cidx + k for k in range(NB)]))
        in_maps.append(m)
    if "nc" not in _NC_CACHE:
        _NC_CACHE["nc"] = build_nc()
    res = run_bass_kernel_spmd(_NC_CACHE["nc"], in_maps, core_ids=list(range(ncores)))
    out = np.empty((ncores * NB, LAT, D), np.float32)
    for cidx in range(ncores):
        oT = res.results[cidx]["outT"]
        for k in range(NB):
            out[NB * cidx + k] = oT[k].T
    return out
```

```python
import math
from contextlib import ExitStack

import numpy as np
import concourse.bass as bass
import concourse.mybir as mybir
from concourse.bass_utils import run_bass_kernel_spmd

F32 = mybir.dt.float32
BF16 = mybir.dt.bfloat16
AF = mybir.ActivationFunctionType
ALU = mybir.AluOpType
AX = mybir.AxisListType

D = 1024
KC = 8
LAT = 2048
NCTX = 256
TOK = LAT + NCTX
NT = TOK // 128
GROUPS = [(0, 512), (512, 512), (1024, 512), (1536, 512), (2048, 256)]
DEPTH = 4
ALPHA = (2.0 * DEPTH) ** 0.25
LN_EPS = 1e-5
RMS_EPS = 1e-5
D_FF = 2816
D_FFE = 3584
NE = 8
SCALE = 64 ** -0.5
NB = 2


class Buf:
    __slots__ = ("w", "r")

    def __init__(self):
        self.w = {}
        self.r = {}


class FW:
    def __init__(self, nc, es):
        self.nc = nc
        self.es = es
        self.eng = {"pe": nc.tensor, "act": nc.scalar, "dve": nc.vector, "pool": nc.gpsimd, "sp": nc.sync}
        self.sem = {}
        self.cnt = {}
        for k in ("pe", "act", "dve"):
            self.sem[k] = es.enter_context(nc.semaphore("s_" + k))
            self.cnt[k] = 0
        self.seen = {k: {} for k in self.eng}
        self.dsem = {}
        self.nwait = 0
        self.ninst = 0

    def new_dsem(self, name):
        s = self.es.enter_context(self.nc.semaphore("d_" + name))
        self.dsem[name] = [s, 0]

    def _wait(self, e, toks):
        eng = self.eng[e]
        seen = self.seen[e]
        for key, (sem, val) in toks.items():
            if seen.get(key, 0) >= val:
                continue
            eng.wait_ge(sem, val)
            self.nwait += 1
            seen[key] = val

    def _deps(self, e, reads, writes):
        toks = {}
        for b in reads:
            for key, t in b.w.items():
                if toks.get(key, (None, 0))[1] < t[1]:
                    toks[key] = t
        for b in writes:
            for d in (b.w, b.r):
                for key, t in d.items():
                    if key == e:
                        continue
                    if toks.get(key, (None, 0))[1] < t[1]:
                        toks[key] = t
        return toks

    def op(self, e, reads, writes, fn):
        self._wait(e, self._deps(e, reads, writes))
        ins = fn()
        self.cnt[e] += 1
        ins.then_inc(self.sem[e], 1)
        tok = (self.sem[e], self.cnt[e])
        for b in writes:
            b.w[e] = tok
        for b in reads:
            b.r[e] = tok
        return ins

    def fence(self, src, dst):
        for d in dst:
            for s_ in src:
                for dd in (s_.w, s_.r):
                    for key, t in dd.items():
                        if d.r.get(key, (None, 0))[1] < t[1]:
                            d.r[key] = t
                        if d.w.get(key, (None, 0))[1] < t[1]:
                            d.w[key] = t

    def dma(self, q, dsem, reads, writes, out, in_):
        self._wait(q, self._deps(q, reads, writes))
        s = self.dsem[dsem]
        ins = self.eng[q].dma_start(out=out, in_=in_)
        s[1] += 16
        ins.then_inc(s[0], 16)
        tok = (s[0], s[1])
        key = "d_" + dsem
        for b in writes:
            b.w[key] = tok
        for b in reads:
            b.r[key] = tok
        return ins


class Rot:
    def __init__(self, items):
        self.items = items
        self.i = 0

    def next(self):
        it = self.items[self.i % len(self.items)]
        self.i += 1
        return it


def lam_init_of(i):
    return 0.8 - 0.6 * math.exp(-0.3 * i)


def build_nc(NL=DEPTH, nb=NB, STAGE=99):
    nc = bass.Bass("TRN2", target_bir_lowering=False)

    def din(name, shape, dt=F32):
        return nc.dram_tensor(name, list(shape), dt, kind="ExternalInput").ap()

    xT_d = din("xT", [nb, D, TOK])
    cT_d = din("cT", [128, KC * 3])
    wmod_d = din("w_mod", [DEPTH, D, 6 * D])
    bmodT_d = din("bmodT", [128, DEPTH * 48])
    lngT_d = din("lngT", [128, DEPTH * 16])
    lnbT_d = din("lnbT", [128, DEPTH * 16])
    wattn_d = din("wattn", [DEPTH, 8, D, 640])
    wo_d = din("wo", [DEPTH, D, D])
    ffg_d = din("ffg", [2, D, D_FF])
    ffu_d = din("ffu", [2, D, D_FF])
    ffd_d = din("ffd", [2, D_FF, D])
    mog_d = din("mog", [2, NE, D, D_FFE])
    mou_d = din("mou", [2, NE, D, D_FFE])
    mod_d = din("mod", [2, NE, D_FFE, D])
    routT_d = din("routT", [128, 2 * KC * NE])
    cos_d = din("cosT", [128, TOK])
    sin_d = din("sinT", [128, TOK])
    mask_d = din("masks", [128, 512])
    sink_d = din("sink", [1, 32])
    lamv_d = din("lamv", [128, 2 * 4 * 64])
    subl_d = din("sublnT", [128, 2])
    ident_d = din("ident", [128, 128])
    outT_d = nc.dram_tensor("outT", [nb, D, LAT], F32, kind="ExternalOutput").ap()
    ascr_d = nc.dram_tensor("a_scr", [8, 128, TOK], BF16, kind=("ExternalOutput" if STAGE < 99 else "Internal")).ap()

    es = ExitStack()
    with es:
        fw = FW(nc, es)
        op = fw.op

        def sb(name, shape, dt):
            return es.enter_context(nc.sbuf_tensor(name, list(shape), dt))

        xT = sb("xT_sb", [128, KC, TOK], F32)
        bx = [Buf() for _ in GROUPS]
        arena = sb("arena", [128, KC * TOK], BF16)
        bh = [Buf() for _ in GROUPS]
        bset = [Buf() for _ in range(2)]
        bset_a = [Buf() for _ in range(2)]

        def hT(kc, t0, n):
            return arena[:, kc * TOK + t0: kc * TOK + t0 + n]

        def setv(s, which):
            base = s * 4 * TOK + {"q": 0, "k": 1, "v": 2, "a": 3}[which] * TOK
            return arena[:, base: base + TOK]

        NSLOT = 8
        wsl = [sb(f"wsl{i}", [128, 2048], BF16) for i in range(NSLOT)]
        bws = [Buf() for _ in range(NSLOT)]
        wctr = [0]
        hb = [sb(f"hb{i}", [128, KC, 512], BF16) for i in range(2)]
        bhb = [Buf() for _ in range(2)]
        rot_hb = Rot(list(zip(hb, bhb)))
        gbc = sb("gbc", [128, TOK], BF16)
        bgbc = Buf()
        sq = [sb(f"sq{i}", [128, 512], F32) for i in range(2)]
        bsq = [Buf() for _ in range(2)]
        rot_sq = Rot(list(zip(sq, bsq)))
        mean_sb = sb("mean_sb", [128, 512], F32); bmean = Buf()
        rstd_sb = sb("rstd_sb", [128, 512], F32); brstd = Buf()
        lt = [sb(f"lt{i}", [128, 512], F32) for i in range(2)]
        blt = [Buf() for _ in range(2)]
        rot_lt = Rot(list(zip(lt, blt)))
        PT = [sb(f"PT{i}", [128, 512], BF16) for i in range(4)]
        bPT = [Buf() for _ in range(4)]
        rot_PT = Rot(list(zip(PT, bPT)))
        nt_ = [sb(f"ntmp{i}", [128, 512], F32) for i in range(3)]
        bnt = [Buf() for _ in range(3)]
        rot_nt = Rot(list(zip(nt_, bnt)))
        st_bf = [sb(f"stbf{i}", [128, 512], F32) for i in range(2)]
        bst = [Buf() for _ in range(2)]
        rot_st = Rot(list(zip(st_bf, bst)))
        cosb = sb("cosb", [128, TOK], BF16); bcos = Buf()
        sinb = sb("sinb", [128, TOK], BF16); bsin = Buf()
        maskb = sb("maskb", [128, 512], BF16); bmask = Buf()
        ident = sb("ident_sb", [128, 128], F32); bident = Buf()
        onesM = sb("onesM", [128, 128], F32); bones = Buf()
        ones128 = sb("ones128", [128, 128], F32)
        onesb = sb("onesb", [128, 128], BF16)
        sinkL = sb("sinkL", [1, 128], BF16)
        sinkv = sb("sinkv", [1, 32], F32); bsinkv = Buf()
        sinkrow = sb("sinkrow", [1, 256], BF16); bsinkrow = Buf()
        lamv = nt_[0]; blamv = bnt[0]
        lamt = sb("lamt", [128, 16], F32); blam = Buf()
        lamscr = sb("lamscr", [128, 64], F32)
        subl = sb("subl_sb", [128, 2], F32); bsubl = Buf()
        cT = sb("cT_sb", [128, KC * 3], F32); bcT = Buf()
        bmodT = sb("bmodT_sb", [128, DEPTH * 48], F32); bbmod = Buf()
        lngT = sb("lngT_sb", [128, DEPTH * 16], F32); blng = Buf()
        lnbT = sb("lnbT_sb", [128, DEPTH * 16], F32); blnb = Buf()
        routT = sb("routT_sb", [128, 2 * KC * NE], F32); brout = Buf()
        modT = sb("modT", [128, DEPTH * 48 * 3], F32); bmod = Buf()
        lg = sb("lg", [128, 32], F32); blg = Buf()
        rt = sb("rt", [128, 64], F32); brt = Buf()
        gates = sb("gates", [128, NT * 8], F32); bgates = Buf()
        gbt = sb("gbt", [128, 128], F32); bgbt = Buf()
        h32 = lt
        bh32 = blt
        rot_h32 = Rot(list(zip(h32, bh32)))

        ps = [es.enter_context(nc.psum_tensor(f"ps{i}", [128, 512], F32)) for i in range(8)]
        bps = [Buf() for _ in range(8)]
        rotS = Rot(list(zip(ps[0:4], bps[0:4])))
        rotA = Rot(list(zip(ps[4:8], bps[4:8])))

        for n in ["const", "constp", "x", "ascr_w", "out", "ar0", "ar1", "wm0", "wm1", "wm2", "wm3"] + [f"w{k}" for k in range(NSLOT)]:
            fw.new_dsem(n)
        rot_hb = Rot([(hb[0], bhb[0], 0), (hb[1], bhb[1], 1)])
        rot_wm = Rot([(h32[0], bh32[0], "wm0"), (h32[1], bh32[1], "wm1"), (sq[0], bsq[0], "wm2"), (sq[1], bsq[1], "wm3")])
        mrow = mean_sb
        bmrow = bmean
        b_ascr = Buf()

        def modcol(i, j, kc, r):
            c = ((i * 48) + j * 8 + kc) * 3 + r
            return modT[:, c: c + 1]

        cb = []

        cbp = []

        def cload(q, dst, src, b):
            if q == "pool":
                fw.dma(q, "constp", [], [b], dst, src)
                cbp.append(b)
            else:
                fw.dma(q, "const", [], [b], dst, src)
                cb.append(b)

        cload("sp", cT[:], cT_d, bcT)
        cload("sp", bmodT[:], bmodT_d, bbmod)
        cload("sp", lngT[:], lngT_d, blng)
        cload("sp", lnbT[:], lnbT_d, blnb)
        cload("sp", routT[:], routT_d, brout)
        cload("sp", ident[:], ident_d, bident)
        cload("sp", sinkv[:], sink_d, bsinkv)
        cload("sp", lamv[:], lamv_d, blamv)
        cload("sp", subl[:], subl_d, bsubl)
        cload("pool", cosb[:], cos_d, bcos)
        cload("pool", sinb[:], sin_d, bsin)
        cload("pool", maskb[:], mask_d, bmask)
        tokc = ("d_const", (fw.dsem["const"][0], fw.dsem["const"][1]))
        for b in cb:
            b.w = {tokc[0]: tokc[1]}
        tokp = ("d_constp", (fw.dsem["constp"][0], fw.dsem["constp"][1]))
        for b in cbp:
            b.w = {tokp[0]: tokp[1]}

        epsc = sb("epsc", [128, 2], F32)

        def _consts():
            nc.vector.memset(epsc[:, 0:1], LN_EPS / ALPHA ** 2)
            nc.vector.memset(epsc[:, 1:2], RMS_EPS)
            nc.vector.memset(onesM[:], 1.0 / 1024.0)
            nc.vector.memset(ones128[:], 1.0 / 128.0)
            nc.vector.memset(onesb[:], 1.0)
            nc.vector.memset(sinkL[:, 0:64], 0.0)
            return nc.vector.memset(sinkL[:, 64:128], 1.0)
        op("dve", [], [bones], _consts)
        op("act", [bsinkv], [bsinkv], lambda: nc.scalar.activation(sinkv[:], sinkv[:], AF.Exp))

        blscr = Buf()
        for j in range(2):
            for t in range(2):
                a = lamv[:, (j * 4 + 2 * t) * 64:(j * 4 + 2 * t + 1) * 64]
                b_ = lamv[:, (j * 4 + 2 * t + 1) * 64:(j * 4 + 2 * t + 2) * 64]
                op("dve", [blamv], [blscr], lambda: nc.vector.tensor_tensor(lamscr[:], a, b_, ALU.mult))
                op("dve", [blscr], [blam],
                   lambda: nc.vector.reduce_sum(lamt[:, j * 4 + t: j * 4 + t + 1], lamscr[:], AX.X))
        for j in range(2):
            op("act", [blam], [blam],
               lambda: nc.scalar.activation(lamt[:, j * 4: j * 4 + 2], lamt[:, j * 4: j * 4 + 2], AF.Exp))
        for j in range(2):
            li = lam_init_of(2 * j + 1)
            op("dve", [blam], [blam],
               lambda: nc.vector.tensor_tensor(lamt[:, j * 4 + 2: j * 4 + 3], lamt[:, j * 4: j * 4 + 1],
                                               lamt[:, j * 4 + 1: j * 4 + 2], ALU.subtract))
            op("dve", [blam], [blam],
               lambda: nc.vector.tensor_scalar(lamt[:, j * 4 + 3: j * 4 + 4], lamt[:, j * 4 + 2: j * 4 + 3],
                                               li, -1.0, op0=ALU.add, op1=ALU.mult))
            op("dve", [bsubl], [bsubl],
               lambda: nc.vector.tensor_scalar(subl[:, j:j + 1], subl[:, j:j + 1], 1.0 - li, None, op0=ALU.mult))

        op("act", [bcT], [bcT], lambda: nc.scalar.activation(cT[:], cT[:], AF.Silu))
        for i in range(NL):
            wv = wmod_d[i].rearrange("(kc p) n -> p kc n", p=128)
            for ncx in range(12):
                pb, bpb = rotA.next()
                for kc in range(KC):
                    t, bt, dn = rot_wm.next()
                    fw.dma("sp", dn, [], [bt], t[:], wv[:, kc, ncx * 512:(ncx + 1) * 512])
                    op("pe", [bcT, bt], [bpb],
                       lambda: nc.tensor.matmul(pb[0:3, :], cT[:, kc * 3:(kc + 1) * 3], t[:],
                                                start=(kc == 0), stop=(kc == KC - 1)))
                op("act", [bpb], [bmrow], lambda: nc.scalar.copy(mrow[0:3, :], pb[0:3, :]))
                tb, btb = rotS.next()

                def _tr():
                    ins = None
                    for q in range(4):
                        ins = nc.tensor.transpose(tb[:, q * 3:(q + 1) * 3], mrow[0:3, q * 128:(q + 1) * 128],
                                                  ident[0:3, 0:3])
                    return ins
                op("pe", [bmrow, bident], [btb], _tr)

                def _ev():
                    ins = None
                    for q in range(4):
                        oc = ncx * 4 + q
                        c = (i * 48 + oc) * 3
                        ins = nc.vector.tensor_scalar(modT[:, c:c + 3], tb[:, q * 3:(q + 1) * 3],
                                                      bmodT[:, i * 48 + oc:i * 48 + oc + 1], None, op0=ALU.add)
                    return ins
                op("dve", [btb, bbmod], [bmod], _ev)

            def _der():
                ins = None
                for j in (1, 4):
                    c = (i * 48 + j * 8) * 3
                    ins = nc.vector.tensor_scalar(modT[:, c:c + 24], modT[:, c:c + 24], 1.0, None, op0=ALU.add)
                for j in (2, 5):
                    c = (i * 48 + j * 8) * 3
                    ins = nc.vector.tensor_scalar(modT[:, c:c + 24], modT[:, c:c + 24], 1.0, 1.0 / ALPHA,
                                                  op0=ALU.add, op1=ALU.mult)
                return ins
            op("dve", [bmod], [bmod], _der)

        def wload(src_ap, a, bb):
            k = wctr[0] % NSLOT
            wctr[0] += 1
            view = wsl[k][:, 0:a * bb].rearrange("p (a b) -> p a b", a=a)
            fw.dma("pool", f"w{k}", [], [bws[k]], view, src_ap)
            return view, bws[k]

        rot_rstd = Rot([(rstd_sb, brstd), (mean_sb, bmean)])

        def ln_stats1(i, s_, g):
            t0, n = GROUPS[g]
            mp = rotA.next()
            qp = rotA.next()
            for kc in range(KC):
                sqt = rot_sq.next()
                op("act", [bx[g]], [sqt[1]],
                   lambda: nc.scalar.activation(sqt[0][:, 0:n], xT[:, kc, t0:t0 + n], AF.Square))

                def _mm():
                    nc.tensor.matmul(mp[0][:, 0:n], onesM[:], xT[:, kc, t0:t0 + n], start=(kc == 0), stop=(kc == KC - 1))
                    return nc.tensor.matmul(qp[0][:, 0:n], onesM[:], sqt[0][:, 0:n], start=(kc == 0), stop=(kc == KC - 1))
                op("pe", [bx[g], sqt[1], bones], [mp[1], qp[1]], _mm)
            return (mp, qp)

        def ln_stats2(i, s_, g, st):
            t0, n = GROUPS[g]
            mp, qp = st
            m2 = rot_lt.next()
            op("act", [mp[1]], [m2[1]], lambda: nc.scalar.activation(m2[0][:, 0:n], mp[0][:, 0:n], AF.Square))
            op("dve", [qp[1], m2[1]], [m2[1]],
               lambda: nc.vector.tensor_tensor(m2[0][:, 0:n], qp[0][:, 0:n], m2[0][:, 0:n], ALU.subtract))
            rs = rot_rstd.next()
            op("act", [m2[1], bones], [rs[1]],
               lambda: nc.scalar.activation(rs[0][:, 0:n], m2[0][:, 0:n], AF.Ln, bias=epsc[:, 0:1], scale=1.0))
            op("act", [rs[1]], [rs[1]],
               lambda: nc.scalar.activation(rs[0][:, 0:n], rs[0][:, 0:n], AF.Exp, scale=-0.5))
            return (mp, rs)

        def ln_apply(i, s_, g, st):
            t0, n = GROUPS[g]
            mp, rs = st
            for kc in range(KC):
                t = rot_lt.next()
                op("dve", [bx[g], mp[1]], [t[1]],
                   lambda: nc.vector.tensor_tensor(t[0][:, 0:n], xT[:, kc, t0:t0 + n], mp[0][:, 0:n], ALU.subtract))
                op("dve", [t[1], rs[1]], [t[1]],
                   lambda: nc.vector.tensor_tensor(t[0][:, 0:n], t[0][:, 0:n], rs[0][:, 0:n], ALU.mult))
                c = (i * 2 + s_) * 8 + kc
                op("act", [t[1], blng, blnb], [bx[g]],
                   lambda: nc.scalar.activation(xT[:, kc, t0:t0 + n], t[0][:, 0:n], AF.Identity,
                                                bias=lnbT[:, c:c + 1], scale=lngT[:, c:c + 1]))

        class LNPipe:
            def __init__(self, i, s_):
                self.i, self.s_, self.prev = i, s_, None

            def push(self, g):
                st1 = ln_stats1(self.i, self.s_, g)
                if self.prev is not None:
                    ln_apply(self.i, self.s_, self.prev[0], self.prev[1])
                st2 = ln_stats2(self.i, self.s_, g, st1)
                self.prev = (g, st2)

            def flush(self):
                if self.prev is not None:
                    ln_apply(self.i, self.s_, self.prev[0], self.prev[1])
                    self.prev = None

        for bi in range(nb):
            def rowof(g):
                return 2 if g == 4 else bi

            for kc in range(KC):
                fw.dma("sp", "x", [], bx, xT[:, kc, :], xT_d[bi, kc * 128:(kc + 1) * 128, :])

            for i in range(NL if STAGE >= 2 else 0):
                with_ctx = i < DEPTH - 1
                isA = (i % 2 == 0)
                j = i // 2
                ngr = 5 if with_ctx else 4
                fw.fence(bh, bset + bset_a)
                wa = wattn_d[i]
                for u in range(8 if STAGE >= 3.7 else 1):
                    s = u % 2
                    qv, kv, vv, av = setv(s, "q"), setv(s, "k"), setv(s, "v"), setv(s, "a")
                    wu_ = wa[u].rearrange("(kc p) n -> p kc n", p=128)
                    wq, bwq = wload(wu_[:, :, 0:256], 8, 256)
                    wk, bwk = wload(wu_[:, :, 256:512], 8, 256)
                    wvv, bwv = wload(wu_[:, :, 512:640], 8, 128)
                    vw = 64 if isA else 128
                    if isA:
                        op("dve", [], [bset[s]],
                           lambda: nc.vector.memset(vv.rearrange("p (t c) -> p t c", c=128)[:, :, 64:128], 1.0))

                        def _sr():
                            nc.vector.tensor_copy(sinkrow[0:1, 0:128],
                                                  sinkv[0:1, j * 16 + 2 * u: j * 16 + 2 * u + 1].to_broadcast([1, 128]))
                            return nc.vector.tensor_copy(sinkrow[0:1, 128:256],
                                                         sinkv[0:1, j * 16 + 2 * u + 1: j * 16 + 2 * u + 2].to_broadcast([1, 128]))
                        op("dve", [bsinkv], [bsinkrow], _sr)
                    def emit_mh(g_):
                        t0_, n_g = GROUPS[g_]
                        r_ = rowof(g_)
                        hb_, bhb_, _ = rot_hb.next()

                        def _mh():
                            ins = None
                            for kc in range(KC):
                                ins = nc.scalar.activation(hb_[:, kc, 0:n_g], xT[:, kc, t0_:t0_ + n_g], AF.Identity,
                                                           bias=modcol(i, 0, kc, r_), scale=modcol(i, 1, kc, r_))
                            return ins
                        op("act", [bx[g_], bmod], [bhb_], _mh)
                        return hb_, bhb_
                    nxt_h = emit_mh(0)
                    for g in range(5):
                        t0, n = GROUPS[g]
                        r = rowof(g)
                        hbt, bhbt = nxt_h
                        for (w, bw, dstv) in ((wq, bwq, qv), (wk, bwk, kv)):
                            p1, bp1 = rotS.next()
                            p2, bp2 = rotS.next()

                            def _mm():
                                ins = None
                                for kc in range(KC):
                                    nc.tensor.matmul(p1[:, 0:n], w[:, kc, 0:128], hbt[:, kc, 0:n],
                                                     start=(kc == 0), stop=(kc == KC - 1))
                                for kc in range(KC):
                                    ins = nc.tensor.matmul(p2[:, 0:n], w[:, kc, 128:256], hbt[:, kc, 0:n],
                                                           start=(kc == 0), stop=(kc == KC - 1))
                                return ins
                            op("pe", [bw, bhbt], [bp1, bp2], _mm)
                            t1, bt1 = rot_nt.next()
                            t2, bt2 = rot_nt.next()
                            op("dve", [bp1, bcos], [bt1],
                               lambda: nc.vector.tensor_tensor(t1[:, 0:n], p1[:, 0:n], cosb[:, t0:t0 + n], ALU.mult))
                            op("dve", [bp2, bsin], [bt2],
                               lambda: nc.vector.tensor_tensor(t2[:, 0:n], p2[:, 0:n], sinb[:, t0:t0 + n], ALU.mult))
                            op("dve", [bt1, bt2], [bset[s]],
                               lambda: nc.vector.tensor_tensor(dstv[:, t0:t0 + n], t1[:, 0:n], t2[:, 0:n], ALU.add))
                        pv, bpv = rotS.next()
                        ntile = n // 128

                        def _mmv():
                            ins = None
                            for tt in range(ntile):
                                for kc in range(KC):
                                    ins = nc.tensor.matmul(pv[:, tt * 128: tt * 128 + vw],
                                                           hbt[:, kc, tt * 128:(tt + 1) * 128], wvv[:, kc, 0:vw],
                                                           start=(kc == 0), stop=(kc == KC - 1))
                            return ins
                        op("pe", [bwv, bhbt], [bpv], _mmv)
                        if g + 1 < 5:
                            nxt_h = emit_mh(g + 1)

                        def _evv():
                            ins = None
                            for tt in range(ntile):
                                o0 = (t0 // 128 + tt) * 128
                                ins = nc.scalar.copy(vv[:, o0:o0 + vw], pv[:, tt * 128: tt * 128 + vw])
                            return ins
                        op("act", [bpv], [bset[s]], _evv)

                    if STAGE < 3:
                        continue
                    if isA:
                        nblocks = 18 if with_ctx else 16
                        items = [(n_, hh) for n_ in range(nblocks) for hh in range(2)]
                        a_state = {}

                        def kts_of(n_):
                            if n_ >= 16:
                                return [(16, None), (17, None)]
                            kts = []
                            if n_ > 0:
                                kts.append((n_ - 1, "prev"))
                            kts.append((n_, None))
                            if n_ < 15:
                                kts.append((n_ + 1, "next"))
                            return kts + [(16, None), (17, None)]

                        def a_stage1(n_, hh):
                            kts = kts_of(n_)
                            nk = len(kts)
                            q0 = n_ * 128
                            pr = slice(64 * hh, 64 * hh + 64)
                            bk = [rotS.next()] + ([rotS.next()] if nk > 4 else [])

                            def _qk():
                                ins = None
                                for idx, (kt, m) in enumerate(kts):
                                    bank = bk[idx // 4][0]
                                    c0 = (idx % 4) * 128
                                    ins = nc.tensor.matmul(bank[:, c0:c0 + 128], kv[pr, kt * 128:(kt + 1) * 128],
                                                           qv[pr, q0:q0 + 128], start=True, stop=True)
                                return ins
                            op("pe", [bset[s]], [b for _, b in bk], _qk)
                            pts = []
                            for bi_, (bank, bbank) in enumerate(bk):
                                width = min(nk - 4 * bi_, 4) * 128
                                pt, bpt = rot_PT.next()
                                op("act", [bbank], [bpt],
                                   lambda: nc.scalar.activation(pt[:, 0:width], bank[:, 0:width], AF.Exp, scale=SCALE))
                                pts.append((pt, bpt))
                            for idx, (kt, m) in enumerate(kts):
                                if m:
                                    pt, bpt = pts[idx // 4]
                                    c0 = (idx % 4) * 128
                                    mo = 0 if m == "prev" else 256
                                    op("dve", [bpt, bmask], [bpt],
                                       lambda: nc.vector.tensor_tensor(pt[:, c0:c0 + 128], pt[:, c0:c0 + 128],
                                                                       maskb[:, mo:mo + 128], ALU.mult))
                            a_state[(n_, hh)] = (kts, pts)

                        def a_stage2(n_, hh):
                            kts, pts = a_state.pop((n_, hh))
                            q0 = n_ * 128
                            acc, bacc = rotA.next()

                            def _pv():
                                for idx, (kt, m) in enumerate(kts):
                                    pt = pts[idx // 4][0]
                                    c0 = (idx % 4) * 128
                                    nc.tensor.matmul(acc[:, 0:128], vv[:, kt * 128:(kt + 1) * 128], pt[:, c0:c0 + 128],
                                                     start=(idx == 0), stop=False)
                                return nc.tensor.matmul(acc[:, 0:128], sinkL[0:1, :], sinkrow[0:1, hh * 128:(hh + 1) * 128],
                                                        start=False, stop=True)
                            op("pe", [bset[s], bsinkrow, bones] + [b for _, b in pts], [bacc], _pv)
                            rc, brc = rot_nt.next()
                            op("act", [bacc], [brc],
                               lambda: nc.scalar.activation(rc[0:64, 0:128], acc[64:128, 0:128], AF.Ln))
                            op("act", [brc], [brc],
                               lambda: nc.scalar.activation(rc[0:64, 0:128], rc[0:64, 0:128], AF.Exp, scale=-1.0))
                            op("dve", [bacc, brc], [bset_a[s]],
                               lambda: nc.vector.tensor_tensor(av[64 * hh:64 * hh + 64, q0:q0 + 128], acc[0:64, 0:128],
                                                               rc[0:64, 0:128], ALU.mult))
                        a_stage1(*items[0])
                        for k_, it in enumerate(items):
                            if k_ + 1 < len(items):
                                a_stage1(*items[k_ + 1])
                            a_stage2(*it)
                    else:
                        o1, o2, d1, d2 = ps[4], ps[5], ps[6], ps[7]
                        accb = [bps[4], bps[5], bps[6], bps[7]]
                        pend2 = [None]
                        for g in range(ngr):
                            t0, n = GROUPS[g]
                            kts = list(range(NT)) if g < 4 else [16, 17]
                            Sb = {}

                            def qk(kt):
                                s1 = rotS.next()
                                s2 = rotS.next()

                                def _f():
                                    nc.tensor.matmul(s1[0][:, 0:n], kv[0:64, kt * 128:(kt + 1) * 128],
                                                     qv[0:64, t0:t0 + n], start=True, stop=True)
                                    return nc.tensor.matmul(s2[0][:, 0:n], kv[64:128, kt * 128:(kt + 1) * 128],
                                                            qv[64:128, t0:t0 + n], start=True, stop=True)
                                op("pe", [bset[s]], [s1[1], s2[1]], _f)
                                p1 = rot_PT.next()
                                p2 = rot_PT.next()
                                op("act", [s1[1]], [p1[1]],
                                   lambda: nc.scalar.activation(p1[0][:, 0:n], s1[0][:, 0:n], AF.Exp, scale=SCALE))
                                op("act", [s2[1]], [p2[1]],
                                   lambda: nc.scalar.activation(p2[0][:, 0:n], s2[0][:, 0:n], AF.Exp, scale=SCALE))
                                Sb[kt] = (p1, p2)
                            qk(kts[0])
                            for idx, kt in enumerate(kts):
                                if idx + 1 < len(kts):
                                    qk(kts[idx + 1])
                                p1, p2 = Sb.pop(kt)
                                st_ = (idx == 0)
                                sp_ = (idx == len(kts) - 1)

                                def _pv():
                                    nc.tensor.matmul(o1[:, 0:n], vv[:, kt * 128:(kt + 1) * 128], p1[0][:, 0:n], start=st_, stop=sp_)
                                    nc.tensor.matmul(d1[:, 0:n], onesb[:], p1[0][:, 0:n], start=st_, stop=sp_)
                                    nc.tensor.matmul(o2[:, 0:n], vv[:, kt * 128:(kt + 1) * 128], p2[0][:, 0:n], start=st_, stop=sp_)
                                    return nc.tensor.matmul(d2[:, 0:n], onesb[:], p2[0][:, 0:n], start=st_, stop=sp_)
                                op("pe", [bset[s], p1[1], p2[1], bones], accb, _pv)
                                if idx == 1 and pend2[0] is not None:
                                    pend2[0]()
                                    pend2[0] = None
                            if pend2[0] is not None:
                                pend2[0]()
                                pend2[0] = None
                            r1 = rot_nt.next()
                            r2 = rot_nt.next()
                            o = rot_nt.next()
                            op("act", [bps[6]], [r1[1]], lambda: nc.scalar.activation(r1[0][:, 0:n], d1[:, 0:n], AF.Ln))
                            op("act", [bps[7]], [r2[1]], lambda: nc.scalar.activation(r2[0][:, 0:n], d2[:, 0:n], AF.Ln))
                            op("act", [r1[1]], [r1[1]],
                               lambda: nc.scalar.activation(r1[0][:, 0:n], r1[0][:, 0:n], AF.Exp, scale=-1.0))
                            op("act", [r2[1]], [r2[1]],
                               lambda: nc.scalar.activation(r2[0][:, 0:n], r2[0][:, 0:n], AF.Exp, scale=-1.0))
                            op("dve", [bps[4], r1[1]], [r1[1]],
                               lambda: nc.vector.tensor_tensor(r1[0][:, 0:n], o1[:, 0:n], r1[0][:, 0:n], ALU.mult))
                            op("dve", [bps[5], r2[1]], [r2[1]],
                               lambda: nc.vector.tensor_tensor(r2[0][:, 0:n], o2[:, 0:n], r2[0][:, 0:n], ALU.mult))
                            def _part2(r1=r1, r2=r2, o=o, n=n, t0=t0):
                              op("dve", [r1[1], r2[1], blam], [o[1]],
                                 lambda: nc.vector.scalar_tensor_tensor(o[0][:, 0:n], r2[0][:, 0:n],
                                                                        lamt[:, j * 4 + 3: j * 4 + 4], r1[0][:, 0:n],
                                                                        op0=ALU.mult, op1=ALU.add))
                              sqt = rot_sq.next()
                              op("act", [o[1]], [sqt[1]],
                                 lambda: nc.scalar.activation(sqt[0][:, 0:n], o[0][:, 0:n], AF.Square))
                              mp = rotS.next()
                              op("pe", [sqt[1], bones], [mp[1]],
                                 lambda: nc.tensor.matmul(mp[0][:, 0:n], ones128[:], sqt[0][:, 0:n], start=True, stop=True))
                              rs = rot_lt.next()
                              op("act", [mp[1], bones], [rs[1]],
                                 lambda: nc.scalar.activation(rs[0][:, 0:n], mp[0][:, 0:n], AF.Ln, bias=epsc[:, 1:2], scale=1.0))
                              op("act", [rs[1]], [rs[1]],
                                 lambda: nc.scalar.activation(rs[0][:, 0:n], rs[0][:, 0:n], AF.Exp, scale=-0.5))
                              op("dve", [o[1], rs[1]], [o[1]],
                                 lambda: nc.vector.tensor_tensor(o[0][:, 0:n], o[0][:, 0:n], rs[0][:, 0:n], ALU.mult))
                              op("act", [o[1], bsubl], [bset_a[s]],
                                 lambda: nc.scalar.activation(av[:, t0:t0 + n], o[0][:, 0:n], AF.Identity,
                                                              scale=subl[:, j:j + 1]))
                            pend2[0] = _part2
                        if pend2[0] is not None:
                            pend2[0]()
                            pend2[0] = None
                    if STAGE >= 3.15:
                        fw.dma("sp", "ascr_w", [bset_a[s]], [b_ascr], ascr_d[u], av)

                if STAGE < 4:
                    continue
                wov = wo_d[i].rearrange("(kc p) n -> p kc n", p=128)
                asv = ascr_d.rearrange("u p t -> p u t")
                lnp = LNPipe(i, 0)
                for g in range(ngr):
                    t0, n = GROUPS[g]
                    r = rowof(g)
                    hbt, bhbt, hk = rot_hb.next()
                    fw.dma("sp", f"ar{hk}", [b_ascr], [bhbt], hbt[:, :, 0:n], asv[:, :, t0:t0 + n])
                    pcs = [wload(wov[:, 2 * q:2 * q + 2, :], 2, 1024) for q in range(4)]
                    for oc in range(KC):
                        bank = rotS.next()

                        def _mm():
                            ins = None
                            for kc in range(KC):
                                ins = nc.tensor.matmul(bank[0][:, 0:n], pcs[kc // 2][0][:, kc % 2, oc * 128:(oc + 1) * 128],
                                                       hbt[:, kc, 0:n], start=(kc == 0), stop=(kc == KC - 1))
                            return ins
                        op("pe", [bhbt] + [p[1] for p in pcs], [bank[1]], _mm)
                        op("dve", [bank[1], bx[g], bmod], [bx[g]],
                           lambda: nc.vector.scalar_tensor_tensor(xT[:, oc, t0:t0 + n], bank[0][:, 0:n],
                                                                  modcol(i, 2, oc, r), xT[:, oc, t0:t0 + n],
                                                                  op0=ALU.mult, op1=ALU.add))
                    lnp.push(g)
                lnp.flush()

                if STAGE < 5 and i == NL - 1:
                    continue
                moe = not isA
                fw.fence(bset + bset_a, bh)
                for g in range(ngr):
                    t0, n = GROUPS[g]
                    r = rowof(g)
                    ntile = n // 128
                    lgp = rotA.next() if moe else None
                    for kc in range(KC):
                        h = rot_h32.next()
                        op("dve", [bx[g], bmod], [h[1]],
                           lambda: nc.vector.tensor_scalar(h[0][:, 0:n], xT[:, kc, t0:t0 + n],
                                                           modcol(i, 4, kc, r), modcol(i, 3, kc, r),
                                                           op0=ALU.mult, op1=ALU.add))
                        op("act", [h[1]], [bh[g]], lambda: nc.scalar.copy(hT(kc, t0, n), h[0][:, 0:n]))
                        if moe:
                            def _r():
                                ins = None
                                for tt in range(ntile):
                                    ins = nc.tensor.matmul(lgp[0][:, tt * 8:(tt + 1) * 8], h[0][:, tt * 128:(tt + 1) * 128],
                                                           routT[:, (j * 8 + kc) * 8:(j * 8 + kc + 1) * 8],
                                                           start=(kc == 0 and tt == 0), stop=(kc == KC - 1))
                                return ins
                            op("pe", [h[1], brout], [lgp[1]], _r)
                    if moe:
                        op("act", [lgp[1]], [blg], lambda: nc.scalar.copy(lg[:, 0:ntile * 8], lgp[0][:, 0:ntile * 8]))
                        for tt in range(ntile):
                            L = lg[:, tt * 8:(tt + 1) * 8]
                            gi = (t0 // 128 + tt) * 8
                            op("dve", [blg], [brt], lambda: nc.vector.max(out=rt[:, 0:8], in_=L))

                            def _b():
                                nc.vector.tensor_scalar(rt[:, 8:9], rt[:, 0:1], -1.0, None, op0=ALU.mult)
                                return nc.vector.tensor_scalar(rt[:, 16:24], L, rt[:, 1:2], None, op0=ALU.is_ge)
                            op("dve", [blg, brt], [brt], _b)
                            op("act", [blg, brt], [brt],
                               lambda: nc.scalar.activation(rt[:, 24:32], L, AF.Exp, bias=rt[:, 8:9], scale=1.0))
                            op("dve", [brt], [brt],
                               lambda: nc.vector.tensor_tensor(rt[:, 32:40], rt[:, 24:32], rt[:, 16:24], ALU.mult))
                            op("dve", [brt], [brt], lambda: nc.vector.reduce_sum(rt[:, 40:41], rt[:, 32:40], AX.X))
                            op("dve", [brt], [brt], lambda: nc.vector.reciprocal(rt[:, 41:42], rt[:, 40:41]))
                            op("dve", [brt], [bgates],
                               lambda: nc.vector.tensor_scalar(gates[:, gi:gi + 8], rt[:, 32:40], rt[:, 41:42], None,
                                                               op0=ALU.mult))
                if moe:
                    experts = list(range(NE))
                    nchunks = D_FFE // 128
                else:
                    experts = [None]
                    nchunks = D_FF // 128
                units = [(c, min(4, nchunks - c)) for c in range(0, nchunks, 4)]
                glist = list(range(ngr))
                for e in experts:
                    if moe:
                        Wg, Wu, Wd = mog_d[j, e], mou_d[j, e], mod_d[j, e]
                        for g in glist:
                            t0, n = GROUPS[g]
                            bank = rotS.next()
                            for tt in range(n // 128):
                                gi = (t0 // 128 + tt) * 8 + e
                                op("dve", [bgates], [bgbt],
                                   lambda: nc.vector.tensor_copy(gbt[:], gates[:, gi:gi + 1].to_broadcast([128, 128])))
                                op("pe", [bgbt, bident], [bank[1]],
                                   lambda: nc.tensor.matmul(bank[0][:, tt * 128:(tt + 1) * 128], gbt[:], ident[:],
                                                            start=True, stop=True))
                            op("act", [bank[1]], [bgbc], lambda: nc.scalar.copy(gbc[:, t0:t0 + n], bank[0][:, 0:n]))
                    else:
                        Wg, Wu, Wd = ffg_d[j], ffu_d[j], ffd_d[j]
                    Wgv = Wg.rearrange("(kc p) n -> p kc n", p=128)
                    Wuv = Wu.rearrange("(kc p) n -> p kc n", p=128)
                    Wdv = Wd.rearrange("(c p) n -> p c n", p=128)
                    for (c0, nch) in units:
                        halves = (nch + 1) // 2
                        wg = [wload(Wgv[:, :, (c0 + 2 * h_) * 128:(c0 + 2 * h_ + 2) * 128], 8, 256) for h_ in range(halves)]
                        wu = [wload(Wuv[:, :, (c0 + 2 * h_) * 128:(c0 + 2 * h_ + 2) * 128], 8, 256) for h_ in range(halves)]
                        wd = [wload(Wdv[:, c0 + 2 * h_: c0 + 2 * h_ + 2, :], 2, 1024) for h_ in range(halves)]
                        pending = None
                        for g in glist + [None]:
                            actb = None
                            if g is not None:
                                t0, n = GROUPS[g]
                                actb = rot_hb.next()
                                for c in range(nch):
                                    gb = rotS.next()
                                    ub = rotS.next()

                                    def _mm():
                                        ins = None
                                        for kc in range(KC):
                                            nc.tensor.matmul(gb[0][:, 0:n], wg[c // 2][0][:, kc, (c % 2) * 128:(c % 2 + 1) * 128],
                                                             hT(kc, t0, n), start=(kc == 0), stop=(kc == KC - 1))
                                        for kc in range(KC):
                                            ins = nc.tensor.matmul(ub[0][:, 0:n], wu[c // 2][0][:, kc, (c % 2) * 128:(c % 2 + 1) * 128],
                                                                   hT(kc, t0, n), start=(kc == 0), stop=(kc == KC - 1))
                                        return ins
                                    op("pe", [bh[g], wg[c // 2][1], wu[c // 2][1]], [gb[1], ub[1]], _mm)
                                    st = rot_st.next()
                                    op("act", [gb[1]], [st[1]],
                                       lambda: nc.scalar.activation(st[0][:, 0:n], gb[0][:, 0:n], AF.Silu))
                                    if moe:
                                        op("dve", [st[1], ub[1]], [st[1]],
                                           lambda: nc.vector.tensor_tensor(st[0][:, 0:n], st[0][:, 0:n], ub[0][:, 0:n], ALU.mult))
                                        op("dve", [st[1], bgbc], [actb[1]],
                                           lambda: nc.vector.tensor_tensor(actb[0][:, c, 0:n], st[0][:, 0:n],
                                                                           gbc[:, t0:t0 + n], ALU.mult))
                                    else:
                                        op("dve", [st[1], ub[1]], [actb[1]],
                                           lambda: nc.vector.tensor_tensor(actb[0][:, c, 0:n], st[0][:, 0:n],
                                                                           ub[0][:, 0:n], ALU.mult))
                            if pending is not None:
                                pg, pact = pending
                                pt0, pn = GROUPS[pg]
                                pr = rowof(pg)
                                for oc in range(KC):
                                    bank = rotA.next()

                                    def _dn():
                                        ins = None
                                        for c in range(nch):
                                            ins = nc.tensor.matmul(bank[0][:, 0:pn],
                                                                   wd[c // 2][0][:, c % 2, oc * 128:(oc + 1) * 128],
                                                                   pact[0][:, c, 0:pn], start=(c == 0), stop=(c == nch - 1))
                                        return ins
                                    op("pe", [pact[1]] + [w_[1] for w_ in wd], [bank[1]], _dn)
                                    op("dve", [bank[1], bx[pg], bmod], [bx[pg]],
                                       lambda: nc.vector.scalar_tensor_tensor(xT[:, oc, pt0:pt0 + pn], bank[0][:, 0:pn],
                                                                              modcol(i, 5, oc, pr), xT[:, oc, pt0:pt0 + pn],
                                                                              op0=ALU.mult, op1=ALU.add))
                            pending = (g, actb) if g is not None else None
                lnp = LNPipe(i, 1)
                for g in glist:
                    lnp.push(g)
                lnp.flush()

            ov = outT_d[bi].rearrange("(kc p) t -> p kc t", p=128)
            for g in range(4):
                t0, n = GROUPS[g]
                fw.dma("sp", "out", [bx[g]], [], ov[:, :, t0:t0 + n], xT[:, :, t0:t0 + n])
        so = fw.dsem["out"]
        nc.sync.wait_ge(so[0], so[1])
        print("build: cnt", fw.cnt, "waits", fw.nwait)
    return nc


def _rope_tables():
    half = 32
    inv = 10000.0 ** (-np.arange(0, half, 2, dtype=np.float32) / half)
    t = np.arange(LAT)
    row = (t // 64).astype(np.float32)
    col = (t % 64).astype(np.float32)
    ang_r = row[:, None] * inv[None, :]
    ang_c = col[:, None] * inv[None, :]
    ang = np.concatenate([ang_r, ang_r, ang_c, ang_c], axis=-1).astype(np.float32)
    cos = np.cos(ang).astype(np.float32)
    sin = np.sin(ang).astype(np.float32)
    sign = np.concatenate([-np.ones(16), np.ones(16), -np.ones(16), np.ones(16)]).astype(np.float32)
    cosT = np.ones((128, TOK), np.float32)
    sinT = np.zeros((128, TOK), np.float32)
    cosT[0:64, 0:LAT] = cos.T
    cosT[64:128, 0:LAT] = cos.T
    sinT[0:64, 0:LAT] = (sin * sign[None, :]).T
    sinT[64:128, 0:LAT] = (sin * sign[None, :]).T
    return cosT, sinT


def _perm128():
    p64 = np.concatenate([np.arange(16, 32), np.arange(0, 16), np.arange(48, 64), np.arange(32, 48)])
    return np.concatenate([p64, 64 + p64])


def _colT(v):
    v = np.asarray(v, np.float32)
    lead = v.shape[:-1]
    n = v.shape[-1] // 128
    a = v.reshape(lead + (n, 128))
    a = np.moveaxis(a, -1, 0)
    return np.ascontiguousarray(a.reshape(128, -1))


def prepare_shared(inp):
    f = lambda k: np.asarray(inp[k], np.float32)
    perm = _perm128()
    a_w = f("a_w_qkv")
    b_w = f("b_w_qkv")
    wattn = np.zeros((DEPTH, 8, D, 640), np.float32)
    for i in range(DEPTH):
        j = i // 2
        for u in range(8):
            if i % 2 == 0:
                W = a_w[j]
                q = W[:, 128 * u:128 * u + 128]
                g = u // 2
                kc = W[:, 1024 + 64 * g: 1024 + 64 * g + 64]
                k = np.concatenate([kc, kc], axis=1)
                v = np.zeros((D, 128), np.float32)
                v[:, 0:64] = W[:, 1280 + 64 * g: 1280 + 64 * g + 64]
            else:
                W = b_w[j]
                q = W[:, 128 * u:128 * u + 128]
                k = W[:, 1024 + 128 * u: 1024 + 128 * u + 128]
                v = W[:, 2048 + 128 * u: 2048 + 128 * u + 128]
            wattn[i, u, :, 0:128] = q
            wattn[i, u, :, 128:256] = q[:, perm]
            wattn[i, u, :, 256:384] = k
            wattn[i, u, :, 384:512] = k[:, perm]
            wattn[i, u, :, 512:640] = v
    wo = np.zeros((DEPTH, D, D), np.float32)
    a_o = f("a_w_o")
    b_o = f("b_w_o")
    for i in range(DEPTH):
        wo[i] = a_o[i // 2] if i % 2 == 0 else b_o[i // 2]
    cosT, sinT = _rope_tables()
    jj = np.arange(128)[:, None]
    ii = np.arange(128)[None, :]
    mprev = (jj >= ii).astype(np.float32)
    mnext = (jj <= ii).astype(np.float32)
    masks = np.concatenate([mprev, mprev, mnext, mnext], axis=1)
    lam = np.stack([np.stack([f("b_lam_q1")[j], f("b_lam_k1")[j], f("b_lam_q2")[j], f("b_lam_k2")[j]]) for j in range(2)])
    lamv = np.ascontiguousarray(np.broadcast_to(lam.reshape(1, -1), (128, 512))).astype(np.float32)
    rout = f("moe_w_router")
    routT = np.ascontiguousarray(rout.reshape(2, KC, 128, NE).transpose(2, 0, 1, 3).reshape(128, -1))
    shared = {
        "w_mod": f("w_mod"),
        "bmodT": _colT(f("b_mod")),
        "lngT": _colT(f("ln_g")),
        "lnbT": _colT(f("ln_b")),
        "wattn": wattn,
        "wo": wo,
        "ffg": f("ff_w_gate"), "ffu": f("ff_w_up"), "ffd": f("ff_w_down"),
        "mog": f("moe_w_gate"), "mou": f("moe_w_up"), "mod": f("moe_w_down"),
        "routT": routT,
        "cosT": cosT, "sinT": sinT, "masks": masks,
        "sink": np.ascontiguousarray(f("a_sink").reshape(1, 32)),
        "lamv": lamv,
        "sublnT": np.ascontiguousarray(f("b_subln_g").T),
        "ident": np.eye(128, dtype=np.float32),
    }
    return shared


def prepare_core(inp, batches):
    x = np.asarray(inp["x"], np.float32)
    ctx = np.asarray(inp["ctx"], np.float32)
    c = np.asarray(inp["c"], np.float32)
    c_ctx = np.asarray(inp["c_ctx"], np.float32)
    xT = np.empty((len(batches), D, TOK), np.float32)
    for k, b in enumerate(batches):
        xT[k, :, 0:LAT] = x[b].T
        xT[k, :, LAT:] = ctx[b].T
    rows = [c[b] for b in batches]
    while len(rows) < 2:
        rows.append(c[batches[0]])
    rows.append(c_ctx)
    rows = np.stack(rows)
    cT = np.ascontiguousarray(rows.reshape(3, KC, 128).transpose(2, 1, 0).reshape(128, KC * 3))
    return {"xT": xT, "cT": cT}


_NC_CACHE = {}


def kernel(**inputs):
    ncores = 8
    shared = prepare_shared(inputs)
    in_maps = []
    for cidx in range(ncores):
        m = dict(shared)
        m.update(prepare_core(inputs, [NB * cidx + k for k in range(NB)]))
        in_maps.append(m)
    if "nc" not in _NC_CACHE:
        _NC_CACHE["nc"] = build_nc()
    res = run_bass_kernel_spmd(_NC_CACHE["nc"], in_maps, core_ids=list(range(ncores)))
    out = np.empty((ncores * NB, LAT, D), np.float32)
    for cidx in range(ncores):
        oT = res.results[cidx]["outT"]
        for k in range(NB):
            out[NB * cidx + k] = oT[k].T
    return out
```

```python
import math
from contextlib import ExitStack

import numpy as np
import concourse.bass as bass
import concourse.mybir as mybir
from concourse.bass_utils import run_bass_kernel_spmd

F32 = mybir.dt.float32
BF16 = mybir.dt.bfloat16
AF = mybir.ActivationFunctionType
ALU = mybir.AluOpType
AX = mybir.AxisListType

D = 1024
KC = 8
LAT = 2048
NCTX = 256
TOK = LAT + NCTX
NT = TOK // 128
GROUPS = [(0, 512), (512, 512), (1024, 512), (1536, 512), (2048, 256)]
DEPTH = 4
ALPHA = (2.0 * DEPTH) ** 0.25
LN_EPS = 1e-5
RMS_EPS = 1e-5
D_FF = 2816
D_FFE = 3584
NE = 8
SCALE = 64 ** -0.5
NB = 2


class Buf:
    __slots__ = ("w", "r")

    def __init__(self):
        self.w = {}
        self.r = {}


class FW:
    def __init__(self, nc, es):
        self.nc = nc
        self.es = es
        self.eng = {"pe": nc.tensor, "act": nc.scalar, "dve": nc.vector, "pool": nc.gpsimd, "sp": nc.sync}
        self.sem = {}
        self.cnt = {}
        for k in ("pe", "act", "dve"):
            self.sem[k] = es.enter_context(nc.semaphore("s_" + k))
            self.cnt[k] = 0
        self.seen = {k: {} for k in self.eng}
        self.dsem = {}
        self.nwait = 0
        self.ninst = 0

    def new_dsem(self, name):
        s = self.es.enter_context(self.nc.semaphore("d_" + name))
        self.dsem[name] = [s, 0]

    def _wait(self, e, toks):
        eng = self.eng[e]
        seen = self.seen[e]
        for key, (sem, val) in toks.items():
            if seen.get(key, 0) >= val:
                continue
            eng.wait_ge(sem, val)
            self.nwait += 1
            seen[key] = val

    def _deps(self, e, reads, writes):
        toks = {}
        for b in reads:
            for key, t in b.w.items():
                if toks.get(key, (None, 0))[1] < t[1]:
                    toks[key] = t
        for b in writes:
            for d in (b.w, b.r):
                for key, t in d.items():
                    if key == e:
                        continue
                    if toks.get(key, (None, 0))[1] < t[1]:
                        toks[key] = t
        return toks

    def op(self, e, reads, writes, fn):
        self._wait(e, self._deps(e, reads, writes))
        ins = fn()
        self.cnt[e] += 1
        ins.then_inc(self.sem[e], 1)
        tok = (self.sem[e], self.cnt[e])
        for b in writes:
            b.w[e] = tok
        for b in reads:
            b.r[e] = tok
        return ins

    def fence(self, src, dst):
        for d in dst:
            for s_ in src:
                for dd in (s_.w, s_.r):
                    for key, t in dd.items():
                        if d.r.get(key, (None, 0))[1] < t[1]:
                            d.r[key] = t
                        if d.w.get(key, (None, 0))[1] < t[1]:
                            d.w[key] = t

    def dma(self, q, dsem, reads, writes, out, in_):
        self._wait(q, self._deps(q, reads, writes))
        s = self.dsem[dsem]
        ins = self.eng[q].dma_start(out=out, in_=in_)
        s[1] += 16
        ins.then_inc(s[0], 16)
        tok = (s[0], s[1])
        key = "d_" + dsem
        for b in writes:
            b.w[key] = tok
        for b in reads:
            b.r[key] = tok
        return ins


class Rot:
    def __init__(self, items):
        self.items = items
        self.i = 0

    def next(self):
        it = self.items[self.i % len(self.items)]
        self.i += 1
        return it


def lam_init_of(i):
    return 0.8 - 0.6 * math.exp(-0.3 * i)


def build_nc(NL=DEPTH, nb=NB, STAGE=99):
    nc = bass.Bass("TRN2", target_bir_lowering=False)

    def din(name, shape, dt=F32):
        return nc.dram_tensor(name, list(shape), dt, kind="ExternalInput").ap()

    xT_d = din("xT", [nb, D, TOK])
    cT_d = din("cT", [128, KC * 3])
    wmod_d = din("w_mod", [DEPTH, D, 6 * D])
    bmodT_d = din("bmodT", [128, DEPTH * 48])
    lngT_d = din("lngT", [128, DEPTH * 16])
    lnbT_d = din("lnbT", [128, DEPTH * 16])
    wattn_d = din("wattn", [DEPTH, 8, D, 640])
    wo_d = din("wo", [DEPTH, D, D])
    ffg_d = din("ffg", [2, D, D_FF])
    ffu_d = din("ffu", [2, D, D_FF])
    ffd_d = din("ffd", [2, D_FF, D])
    mog_d = din("mog", [2, NE, D, D_FFE])
    mou_d = din("mou", [2, NE, D, D_FFE])
    mod_d = din("mod", [2, NE, D_FFE, D])
    routT_d = din("routT", [128, 2 * KC * NE])
    cos_d = din("cosT", [128, TOK])
    sin_d = din("sinT", [128, TOK])
    mask_d = din("masks", [128, 512])
    sink_d = din("sink", [1, 32])
    lamv_d = din("lamv", [128, 2 * 4 * 64])
    subl_d = din("sublnT", [128, 2])
    ident_d = din("ident", [128, 128])
    outT_d = nc.dram_tensor("outT", [nb, D, LAT], F32, kind="ExternalOutput").ap()
    ascr_d = nc.dram_tensor("a_scr", [8, 128, TOK], BF16, kind=("ExternalOutput" if STAGE < 99 else "Internal")).ap()

    es = ExitStack()
    with es:
        fw = FW(nc, es)
        op = fw.op

        def sb(name, shape, dt):
            return es.enter_context(nc.sbuf_tensor(name, list(shape), dt))

        xT = sb("xT_sb", [128, KC, TOK], F32)
        bx = [Buf() for _ in GROUPS]
        arena = sb("arena", [128, KC * TOK], BF16)
        bh = [Buf() for _ in GROUPS]
        bset = [Buf() for _ in range(2)]
        bset_a = [Buf() for _ in range(2)]

        def hT(kc, t0, n):
            return arena[:, kc * TOK + t0: kc * TOK + t0 + n]

        def setv(s, which):
            base = s * 4 * TOK + {"q": 0, "k": 1, "v": 2, "a": 3}[which] * TOK
            return arena[:, base: base + TOK]

        NSLOT = 8
        wsl = [sb(f"wsl{i}", [128, 2048], BF16) for i in range(NSLOT)]
        bws = [Buf() for _ in range(NSLOT)]
        wctr = [0]
        hb = [sb(f"hb{i}", [128, KC, 512], BF16) for i in range(2)]
        bhb = [Buf() for _ in range(2)]
        rot_hb = Rot(list(zip(hb, bhb)))
        gbc = sb("gbc", [128, TOK], BF16)
        bgbc = Buf()
        sq = [sb(f"sq{i}", [128, 512], F32) for i in range(2)]
        bsq = [Buf() for _ in range(2)]
        rot_sq = Rot(list(zip(sq, bsq)))
        mean_sb = sb("mean_sb", [128, 512], F32); bmean = Buf()
        rstd_sb = sb("rstd_sb", [128, 512], F32); brstd = Buf()
        lt = [sb(f"lt{i}", [128, 512], F32) for i in range(2)]
        blt = [Buf() for _ in range(2)]
        rot_lt = Rot(list(zip(lt, blt)))
        PT = [sb(f"PT{i}", [128, 512], BF16) for i in range(4)]
        bPT = [Buf() for _ in range(4)]
        rot_PT = Rot(list(zip(PT, bPT)))
        nt_ = [sb(f"ntmp{i}", [128, 512], F32) for i in range(3)]
        bnt = [Buf() for _ in range(3)]
        rot_nt = Rot(list(zip(nt_, bnt)))
        st_bf = [sb(f"stbf{i}", [128, 512], F32) for i in range(2)]
        bst = [Buf() for _ in range(2)]
        rot_st = Rot(list(zip(st_bf, bst)))
        cosb = sb("cosb", [128, TOK], BF16); bcos = Buf()
        sinb = sb("sinb", [128, TOK], BF16); bsin = Buf()
        maskb = sb("maskb", [128, 512], BF16); bmask = Buf()
        ident = sb("ident_sb", [128, 128], F32); bident = Buf()
        onesM = sb("onesM", [128, 128], F32); bones = Buf()
        ones128 = sb("ones128", [128, 128], F32)
        onesb = sb("onesb", [128, 128], BF16)
        sinkL = sb("sinkL", [1, 128], BF16)
        sinkv = sb("sinkv", [1, 32], F32); bsinkv = Buf()
        sinkrow = sb("sinkrow", [1, 256], BF16); bsinkrow = Buf()
        lamv = nt_[0]; blamv = bnt[0]
        lamt = sb("lamt", [128, 16], F32); blam = Buf()
        lamscr = sb("lamscr", [128, 64], F32)
        subl = sb("subl_sb", [128, 2], F32); bsubl = Buf()
        cT = sb("cT_sb", [128, KC * 3], F32); bcT = Buf()
        bmodT = sb("bmodT_sb", [128, DEPTH * 48], F32); bbmod = Buf()
        lngT = sb("lngT_sb", [128, DEPTH * 16], F32); blng = Buf()
        lnbT = sb("lnbT_sb", [128, DEPTH * 16], F32); blnb = Buf()
        routT = sb("routT_sb", [128, 2 * KC * NE], F32); brout = Buf()
        modT = sb("modT", [128, DEPTH * 48 * 3], F32); bmod = Buf()
        lg = sb("lg", [128, 32], F32); blg = Buf()
        rt = sb("rt", [128, 4 * 48], F32); brt = [Buf() for _ in range(4)]
        gates = sb("gates", [128, NT * 8], F32); bgates = Buf()
        gbt = sb("gbt", [128, 128], F32); bgbt = Buf()
        h32 = lt
        bh32 = blt
        rot_h32 = Rot(list(zip(h32, bh32)))

        ps = [es.enter_context(nc.psum_tensor(f"ps{i}", [128, 512], F32)) for i in range(8)]
        bps = [Buf() for _ in range(8)]
        rotS = Rot(list(zip(ps[0:4], bps[0:4])))
        rotA = Rot(list(zip(ps[4:8], bps[4:8])))

        for n in ["const", "constp", "x", "ascr_w", "out", "ar0", "ar1", "wm0", "wm1", "wm2", "wm3"] + [f"w{k}" for k in range(NSLOT)]:
            fw.new_dsem(n)
        rot_hb = Rot([(hb[0], bhb[0], 0), (hb[1], bhb[1], 1)])
        rot_wm = Rot([(h32[0], bh32[0], "wm0"), (h32[1], bh32[1], "wm1"), (sq[0], bsq[0], "wm2"), (sq[1], bsq[1], "wm3")])
        mrow = mean_sb
        bmrow = bmean
        b_ascr = Buf()

        def modcol(i, j, kc, r):
            c = ((i * 48) + j * 8 + kc) * 3 + r
            return modT[:, c: c + 1]

        cb = []

        cbp = []

        def cload(q, dst, src, b):
            if q == "pool":
                fw.dma(q, "constp", [], [b], dst, src)
                cbp.append(b)
            else:
                fw.dma(q, "const", [], [b], dst, src)
                cb.append(b)

        cload("sp", cT[:], cT_d, bcT)
        cload("sp", bmodT[:], bmodT_d, bbmod)
        cload("sp", lngT[:], lngT_d, blng)
        cload("sp", lnbT[:], lnbT_d, blnb)
        cload("sp", routT[:], routT_d, brout)
        cload("sp", ident[:], ident_d, bident)
        cload("sp", sinkv[:], sink_d, bsinkv)
        cload("sp", lamv[:], lamv_d, blamv)
        cload("sp", subl[:], subl_d, bsubl)
        cload("pool", cosb[:], cos_d, bcos)
        cload("pool", sinb[:], sin_d, bsin)
        cload("pool", maskb[:], mask_d, bmask)
        tokc = ("d_const", (fw.dsem["const"][0], fw.dsem["const"][1]))
        for b in cb:
            b.w = {tokc[0]: tokc[1]}
        tokp = ("d_constp", (fw.dsem["constp"][0], fw.dsem["constp"][1]))
        for b in cbp:
            b.w = {tokp[0]: tokp[1]}

        epsc = sb("epsc", [128, 2], F32)

        def _consts():
            nc.vector.memset(epsc[:, 0:1], LN_EPS / ALPHA ** 2)
            nc.vector.memset(epsc[:, 1:2], RMS_EPS)
            nc.vector.memset(onesM[:], 1.0 / 1024.0)
            nc.vector.memset(ones128[:], 1.0 / 128.0)
            nc.vector.memset(onesb[:], 1.0)
            nc.vector.memset(sinkL[:, 0:64], 0.0)
            return nc.vector.memset(sinkL[:, 64:128], 1.0)
        op("dve", [], [bones], _consts)
        op("act", [bsinkv], [bsinkv], lambda: nc.scalar.activation(sinkv[:], sinkv[:], AF.Exp))

        blscr = Buf()
        for j in range(2):
            for t in range(2):
                a = lamv[:, (j * 4 + 2 * t) * 64:(j * 4 + 2 * t + 1) * 64]
                b_ = lamv[:, (j * 4 + 2 * t + 1) * 64:(j * 4 + 2 * t + 2) * 64]
                op("dve", [blamv], [blscr], lambda: nc.vector.tensor_tensor(lamscr[:], a, b_, ALU.mult))
                op("dve", [blscr], [blam],
                   lambda: nc.vector.reduce_sum(lamt[:, j * 4 + t: j * 4 + t + 1], lamscr[:], AX.X))
        for j in range(2):
            op("act", [blam], [blam],
               lambda: nc.scalar.activation(lamt[:, j * 4: j * 4 + 2], lamt[:, j * 4: j * 4 + 2], AF.Exp))
        for j in range(2):
            li = lam_init_of(2 * j + 1)
            op("dve", [blam], [blam],
               lambda: nc.vector.tensor_tensor(lamt[:, j * 4 + 2: j * 4 + 3], lamt[:, j * 4: j * 4 + 1],
                                               lamt[:, j * 4 + 1: j * 4 + 2], ALU.subtract))
            op("dve", [blam], [blam],
               lambda: nc.vector.tensor_scalar(lamt[:, j * 4 + 3: j * 4 + 4], lamt[:, j * 4 + 2: j * 4 + 3],
                                               li, -1.0, op0=ALU.add, op1=ALU.mult))
            op("dve", [bsubl], [bsubl],
               lambda: nc.vector.tensor_scalar(subl[:, j:j + 1], subl[:, j:j + 1], 1.0 - li, None, op0=ALU.mult))

        op("act", [bcT], [bcT], lambda: nc.scalar.activation(cT[:], cT[:], AF.Silu))
        for i in range(NL):
            wv = wmod_d[i].rearrange("(kc p) n -> p kc n", p=128)
            for ncx in range(12):
                pb, bpb = rotA.next()
                for kc in range(KC):
                    t, bt, dn = rot_wm.next()
                    fw.dma("sp", dn, [], [bt], t[:], wv[:, kc, ncx * 512:(ncx + 1) * 512])
                    op("pe", [bcT, bt], [bpb],
                       lambda: nc.tensor.matmul(pb[0:3, :], cT[:, kc * 3:(kc + 1) * 3], t[:],
                                                start=(kc == 0), stop=(kc == KC - 1)))
                op("act", [bpb], [bmrow], lambda: nc.scalar.copy(mrow[0:3, :], pb[0:3, :]))
                tb, btb = rotS.next()

                def _tr():
                    ins = None
                    for q in range(4):
                        ins = nc.tensor.transpose(tb[:, q * 3:(q + 1) * 3], mrow[0:3, q * 128:(q + 1) * 128],
                                                  ident[0:3, 0:3])
                    return ins
                op("pe", [bmrow, bident], [btb], _tr)

                def _ev():
                    ins = None
                    for q in range(4):
                        oc = ncx * 4 + q
                        c = (i * 48 + oc) * 3
                        ins = nc.vector.tensor_scalar(modT[:, c:c + 3], tb[:, q * 3:(q + 1) * 3],
                                                      bmodT[:, i * 48 + oc:i * 48 + oc + 1], None, op0=ALU.add)
                    return ins
                op("dve", [btb, bbmod], [bmod], _ev)

            def _der():
                ins = None
                for j in (1, 4):
                    c = (i * 48 + j * 8) * 3
                    ins = nc.vector.tensor_scalar(modT[:, c:c + 24], modT[:, c:c + 24], 1.0, None, op0=ALU.add)
                for j in (2, 5):
                    c = (i * 48 + j * 8) * 3
                    ins = nc.vector.tensor_scalar(modT[:, c:c + 24], modT[:, c:c + 24], 1.0, 1.0 / ALPHA,
                                                  op0=ALU.add, op1=ALU.mult)
                return ins
            op("dve", [bmod], [bmod], _der)

        def wload(src_ap, a, bb):
            k = wctr[0] % NSLOT
            wctr[0] += 1
            view = wsl[k][:, 0:a * bb].rearrange("p (a b) -> p a b", a=a)
            fw.dma("pool", f"w{k}", [], [bws[k]], view, src_ap)
            return view, bws[k]

        rot_rstd = Rot([(rstd_sb, brstd), (mean_sb, bmean)])

        def ln_stats1(i, s_, g):
            t0, n = GROUPS[g]
            mp = rotA.next()
            qp = rotA.next()
            for kc in range(KC):
                sqt = rot_sq.next()
                op("act", [bx[g]], [sqt[1]],
                   lambda: nc.scalar.activation(sqt[0][:, 0:n], xT[:, kc, t0:t0 + n], AF.Square))

                def _mm():
                    nc.tensor.matmul(mp[0][:, 0:n], onesM[:], xT[:, kc, t0:t0 + n], start=(kc == 0), stop=(kc == KC - 1))
                    return nc.tensor.matmul(qp[0][:, 0:n], onesM[:], sqt[0][:, 0:n], start=(kc == 0), stop=(kc == KC - 1))
                op("pe", [bx[g], sqt[1], bones], [mp[1], qp[1]], _mm)
            return (mp, qp)

        def ln_stats2(i, s_, g, st):
            t0, n = GROUPS[g]
            mp, qp = st
            m2 = rot_lt.next()
            op("act", [mp[1]], [m2[1]], lambda: nc.scalar.activation(m2[0][:, 0:n], mp[0][:, 0:n], AF.Square))
            op("dve", [qp[1], m2[1]], [m2[1]],
               lambda: nc.vector.tensor_tensor(m2[0][:, 0:n], qp[0][:, 0:n], m2[0][:, 0:n], ALU.subtract))
            rs = rot_rstd.next()
            op("act", [m2[1], bones], [rs[1]],
               lambda: nc.scalar.activation(rs[0][:, 0:n], m2[0][:, 0:n], AF.Ln, bias=epsc[:, 0:1], scale=1.0))
            op("act", [rs[1]], [rs[1]],
               lambda: nc.scalar.activation(rs[0][:, 0:n], rs[0][:, 0:n], AF.Exp, scale=-0.5))
            return (mp, rs)

        def ln_apply(i, s_, g, st):
            t0, n = GROUPS[g]
            mp, rs = st
            for kc in range(KC):
                t = rot_lt.next()
                op("dve", [bx[g], mp[1]], [t[1]],
                   lambda: nc.vector.tensor_tensor(t[0][:, 0:n], xT[:, kc, t0:t0 + n], mp[0][:, 0:n], ALU.subtract))
                op("dve", [t[1], rs[1]], [t[1]],
                   lambda: nc.vector.tensor_tensor(t[0][:, 0:n], t[0][:, 0:n], rs[0][:, 0:n], ALU.mult))
                c = (i * 2 + s_) * 8 + kc
                op("act", [t[1], blng, blnb], [bx[g]],
                   lambda: nc.scalar.activation(xT[:, kc, t0:t0 + n], t[0][:, 0:n], AF.Identity,
                                                bias=lnbT[:, c:c + 1], scale=lngT[:, c:c + 1]))

        class LNPipe:
            def __init__(self, i, s_):
                self.i, self.s_, self.prev = i, s_, None

            def push(self, g):
                st1 = ln_stats1(self.i, self.s_, g)
                if self.prev is not None:
                    ln_apply(self.i, self.s_, self.prev[0], self.prev[1])
                st2 = ln_stats2(self.i, self.s_, g, st1)
                self.prev = (g, st2)

            def flush(self):
                if self.prev is not None:
                    ln_apply(self.i, self.s_, self.prev[0], self.prev[1])
                    self.prev = None

        for bi in range(nb):
            def rowof(g):
                return 2 if g == 4 else bi

            for kc in range(KC):
                fw.dma("sp", "x", [], bx, xT[:, kc, :], xT_d[bi, kc * 128:(kc + 1) * 128, :])

            for i in range(NL if STAGE >= 2 else 0):
                with_ctx = i < DEPTH - 1
                isA = (i % 2 == 0)
                j = i // 2
                ngr = 5 if with_ctx else 4
                fw.fence(bh, bset + bset_a)
                wa = wattn_d[i]
                for u in range(8 if STAGE >= 3.7 else 1):
                    s = u % 2
                    qv, kv, vv, av = setv(s, "q"), setv(s, "k"), setv(s, "v"), setv(s, "a")
                    wu_ = wa[u].rearrange("(kc p) n -> p kc n", p=128)
                    wq, bwq = wload(wu_[:, :, 0:256], 8, 256)
                    wk, bwk = wload(wu_[:, :, 256:512], 8, 256)
                    wvv, bwv = wload(wu_[:, :, 512:640], 8, 128)
                    vw = 64 if isA else 128
                    if isA:
                        op("dve", [], [bset[s]],
                           lambda: nc.vector.memset(vv.rearrange("p (t c) -> p t c", c=128)[:, :, 64:128], 1.0))

                        def _sr():
                            nc.vector.tensor_copy(sinkrow[0:1, 0:128],
                                                  sinkv[0:1, j * 16 + 2 * u: j * 16 + 2 * u + 1].to_broadcast([1, 128]))
                            return nc.vector.tensor_copy(sinkrow[0:1, 128:256],
                                                         sinkv[0:1, j * 16 + 2 * u + 1: j * 16 + 2 * u + 2].to_broadcast([1, 128]))
                        op("dve", [bsinkv], [bsinkrow], _sr)
                    def emit_mh(g_):
                        t0_, n_g = GROUPS[g_]
                        r_ = rowof(g_)
                        hb_, bhb_, _ = rot_hb.next()

                        def _mh():
                            ins = None
                            for kc in range(KC):
                                ins = nc.scalar.activation(hb_[:, kc, 0:n_g], xT[:, kc, t0_:t0_ + n_g], AF.Identity,
                                                           bias=modcol(i, 0, kc, r_), scale=modcol(i, 1, kc, r_))
                            return ins
                        op("act", [bx[g_], bmod], [bhb_], _mh)
                        return hb_, bhb_
                    nxt_h = emit_mh(0)
                    for g in range(5):
                        t0, n = GROUPS[g]
                        r = rowof(g)
                        hbt, bhbt = nxt_h
                        for (w, bw, dstv) in ((wq, bwq, qv), (wk, bwk, kv)):
                            p1, bp1 = rotS.next()
                            p2, bp2 = rotS.next()

                            def _mm():
                                ins = None
                                for kc in range(KC):
                                    nc.tensor.matmul(p1[:, 0:n], w[:, kc, 0:128], hbt[:, kc, 0:n],
                                                     start=(kc == 0), stop=(kc == KC - 1))
                                for kc in range(KC):
                                    ins = nc.tensor.matmul(p2[:, 0:n], w[:, kc, 128:256], hbt[:, kc, 0:n],
                                                           start=(kc == 0), stop=(kc == KC - 1))
                                return ins
                            op("pe", [bw, bhbt], [bp1, bp2], _mm)
                            t1, bt1 = rot_nt.next()
                            t2, bt2 = rot_nt.next()
                            op("dve", [bp1, bcos], [bt1],
                               lambda: nc.vector.tensor_tensor(t1[:, 0:n], p1[:, 0:n], cosb[:, t0:t0 + n], ALU.mult))
                            op("dve", [bp2, bsin], [bt2],
                               lambda: nc.vector.tensor_tensor(t2[:, 0:n], p2[:, 0:n], sinb[:, t0:t0 + n], ALU.mult))
                            op("dve", [bt1, bt2], [bset[s]],
                               lambda: nc.vector.tensor_tensor(dstv[:, t0:t0 + n], t1[:, 0:n], t2[:, 0:n], ALU.add))
                        pv, bpv = rotS.next()
                        ntile = n // 128

                        def _mmv():
                            ins = None
                            for tt in range(ntile):
                                for kc in range(KC):
                                    ins = nc.tensor.matmul(pv[:, tt * 128: tt * 128 + vw],
                                                           hbt[:, kc, tt * 128:(tt + 1) * 128], wvv[:, kc, 0:vw],
                                                           start=(kc == 0), stop=(kc == KC - 1))
                            return ins
                        op("pe", [bwv, bhbt], [bpv], _mmv)
                        if g + 1 < 5:
                            nxt_h = emit_mh(g + 1)

                        def _evv():
                            ins = None
                            for tt in range(ntile):
                                o0 = (t0 // 128 + tt) * 128
                                ins = nc.scalar.copy(vv[:, o0:o0 + vw], pv[:, tt * 128: tt * 128 + vw])
                            return ins
                        op("act", [bpv], [bset[s]], _evv)

                    if STAGE < 3:
                        continue
                    if isA:
                        nblocks = 18 if with_ctx else 16
                        items = [(n_, hh) for n_ in range(nblocks) for hh in range(2)]
                        a_state = {}

                        def kts_of(n_):
                            if n_ >= 16:
                                return [(16, None), (17, None)]
                            kts = []
                            if n_ > 0:
                                kts.append((n_ - 1, "prev"))
                            kts.append((n_, None))
                            if n_ < 15:
                                kts.append((n_ + 1, "next"))
                            return kts + [(16, None), (17, None)]

                        def a_stage1(n_, hh):
                            kts = kts_of(n_)
                            nk = len(kts)
                            q0 = n_ * 128
                            pr = slice(64 * hh, 64 * hh + 64)
                            bk = [rotS.next()] + ([rotS.next()] if nk > 4 else [])

                            def _qk():
                                ins = None
                                for idx, (kt, m) in enumerate(kts):
                                    bank = bk[idx // 4][0]
                                    c0 = (idx % 4) * 128
                                    ins = nc.tensor.matmul(bank[:, c0:c0 + 128], kv[pr, kt * 128:(kt + 1) * 128],
                                                           qv[pr, q0:q0 + 128], start=True, stop=True)
                                return ins
                            op("pe", [bset[s]], [b for _, b in bk], _qk)
                            pts = []
                            for bi_, (bank, bbank) in enumerate(bk):
                                width = min(nk - 4 * bi_, 4) * 128
                                pt, bpt = rot_PT.next()
                                op("act", [bbank], [bpt],
                                   lambda: nc.scalar.activation(pt[:, 0:width], bank[:, 0:width], AF.Exp, scale=SCALE))
                                pts.append((pt, bpt))
                            for idx, (kt, m) in enumerate(kts):
                                if m:
                                    pt, bpt = pts[idx // 4]
                                    c0 = (idx % 4) * 128
                                    mo = 0 if m == "prev" else 256
                                    op("dve", [bpt, bmask], [bpt],
                                       lambda: nc.vector.tensor_tensor(pt[:, c0:c0 + 128], pt[:, c0:c0 + 128],
                                                                       maskb[:, mo:mo + 128], ALU.mult))
                            a_state[(n_, hh)] = (kts, pts)

                        def a_stage2(n_, hh):
                            kts, pts = a_state.pop((n_, hh))
                            q0 = n_ * 128
                            acc, bacc = rotA.next()

                            def _pv():
                                for idx, (kt, m) in enumerate(kts):
                                    pt = pts[idx // 4][0]
                                    c0 = (idx % 4) * 128
                                    nc.tensor.matmul(acc[:, 0:128], vv[:, kt * 128:(kt + 1) * 128], pt[:, c0:c0 + 128],
                                                     start=(idx == 0), stop=False)
                                return nc.tensor.matmul(acc[:, 0:128], sinkL[0:1, :], sinkrow[0:1, hh * 128:(hh + 1) * 128],
                                                        start=False, stop=True)
                            op("pe", [bset[s], bsinkrow, bones] + [b for _, b in pts], [bacc], _pv)
                            rc, brc = rot_nt.next()
                            op("act", [bacc], [brc],
                               lambda: nc.scalar.activation(rc[0:64, 0:128], acc[64:128, 0:128], AF.Ln))
                            op("act", [brc], [brc],
                               lambda: nc.scalar.activation(rc[0:64, 0:128], rc[0:64, 0:128], AF.Exp, scale=-1.0))
                            op("dve", [bacc, brc], [bset_a[s]],
                               lambda: nc.vector.tensor_tensor(av[64 * hh:64 * hh + 64, q0:q0 + 128], acc[0:64, 0:128],
                                                               rc[0:64, 0:128], ALU.mult))
                        a_stage1(*items[0])
                        for k_, it in enumerate(items):
                            if k_ + 1 < len(items):
                                a_stage1(*items[k_ + 1])
                            a_stage2(*it)
                    else:
                        o1, o2, d1, d2 = ps[4], ps[5], ps[6], ps[7]
                        accb = [bps[4], bps[5], bps[6], bps[7]]
                        pend2 = [None]
                        for g in range(ngr):
                            t0, n = GROUPS[g]
                            kts = list(range(NT)) if g < 4 else [16, 17]
                            Sb = {}

                            def qk(kt):
                                s1 = rotS.next()
                                s2 = rotS.next()

                                def _f():
                                    nc.tensor.matmul(s1[0][:, 0:n], kv[0:64, kt * 128:(kt + 1) * 128],
                                                     qv[0:64, t0:t0 + n], start=True, stop=True)
                                    return nc.tensor.matmul(s2[0][:, 0:n], kv[64:128, kt * 128:(kt + 1) * 128],
                                                            qv[64:128, t0:t0 + n], start=True, stop=True)
                                op("pe", [bset[s]], [s1[1], s2[1]], _f)
                                p1 = rot_PT.next()
                                p2 = rot_PT.next()
                                op("act", [s1[1]], [p1[1]],
                                   lambda: nc.scalar.activation(p1[0][:, 0:n], s1[0][:, 0:n], AF.Exp, scale=SCALE))
                                op("act", [s2[1]], [p2[1]],
                                   lambda: nc.scalar.activation(p2[0][:, 0:n], s2[0][:, 0:n], AF.Exp, scale=SCALE))
                                Sb[kt] = (p1, p2)
                            qk(kts[0])
                            for idx, kt in enumerate(kts):
                                if idx + 1 < len(kts):
                                    qk(kts[idx + 1])
                                p1, p2 = Sb.pop(kt)
                                st_ = (idx == 0)
                                sp_ = (idx == len(kts) - 1)

                                def _pv():
                                    nc.tensor.matmul(o1[:, 0:n], vv[:, kt * 128:(kt + 1) * 128], p1[0][:, 0:n], start=st_, stop=sp_)
                                    nc.tensor.matmul(d1[:, 0:n], onesb[:], p1[0][:, 0:n], start=st_, stop=sp_)
                                    nc.tensor.matmul(o2[:, 0:n], vv[:, kt * 128:(kt + 1) * 128], p2[0][:, 0:n], start=st_, stop=sp_)
                                    return nc.tensor.matmul(d2[:, 0:n], onesb[:], p2[0][:, 0:n], start=st_, stop=sp_)
                                op("pe", [bset[s], p1[1], p2[1], bones], accb, _pv)
                                if idx == 1 and pend2[0] is not None:
                                    pend2[0]()
                                    pend2[0] = None
                            if pend2[0] is not None:
                                pend2[0]()
                                pend2[0] = None
                            r1 = rot_nt.next()
                            r2 = rot_nt.next()
                            o = rot_nt.next()
                            op("act", [bps[6]], [r1[1]], lambda: nc.scalar.activation(r1[0][:, 0:n], d1[:, 0:n], AF.Ln))
                            op("act", [bps[7]], [r2[1]], lambda: nc.scalar.activation(r2[0][:, 0:n], d2[:, 0:n], AF.Ln))
                            op("act", [r1[1]], [r1[1]],
                               lambda: nc.scalar.activation(r1[0][:, 0:n], r1[0][:, 0:n], AF.Exp, scale=-1.0))
                            op("act", [r2[1]], [r2[1]],
                               lambda: nc.scalar.activation(r2[0][:, 0:n], r2[0][:, 0:n], AF.Exp, scale=-1.0))
                            op("dve", [bps[4], r1[1]], [r1[1]],
                               lambda: nc.vector.tensor_tensor(r1[0][:, 0:n], o1[:, 0:n], r1[0][:, 0:n], ALU.mult))
                            op("dve", [bps[5], r2[1]], [r2[1]],
                               lambda: nc.vector.tensor_tensor(r2[0][:, 0:n], o2[:, 0:n], r2[0][:, 0:n], ALU.mult))
                            def _part2(r1=r1, r2=r2, o=o, n=n, t0=t0):
                              op("dve", [r1[1], r2[1], blam], [o[1]],
                                 lambda: nc.vector.scalar_tensor_tensor(o[0][:, 0:n], r2[0][:, 0:n],
                                                                        lamt[:, j * 4 + 3: j * 4 + 4], r1[0][:, 0:n],
                                                                        op0=ALU.mult, op1=ALU.add))
                              sqt = rot_sq.next()
                              op("act", [o[1]], [sqt[1]],
                                 lambda: nc.scalar.activation(sqt[0][:, 0:n], o[0][:, 0:n], AF.Square))
                              mp = rotS.next()
                              op("pe", [sqt[1], bones], [mp[1]],
                                 lambda: nc.tensor.matmul(mp[0][:, 0:n], ones128[:], sqt[0][:, 0:n], start=True, stop=True))
                              rs = rot_lt.next()
                              op("act", [mp[1], bones], [rs[1]],
                                 lambda: nc.scalar.activation(rs[0][:, 0:n], mp[0][:, 0:n], AF.Ln, bias=epsc[:, 1:2], scale=1.0))
                              op("act", [rs[1]], [rs[1]],
                                 lambda: nc.scalar.activation(rs[0][:, 0:n], rs[0][:, 0:n], AF.Exp, scale=-0.5))
                              op("dve", [o[1], rs[1]], [o[1]],
                                 lambda: nc.vector.tensor_tensor(o[0][:, 0:n], o[0][:, 0:n], rs[0][:, 0:n], ALU.mult))
                              op("act", [o[1], bsubl], [bset_a[s]],
                                 lambda: nc.scalar.activation(av[:, t0:t0 + n], o[0][:, 0:n], AF.Identity,
                                                              scale=subl[:, j:j + 1]))
                            pend2[0] = _part2
                        if pend2[0] is not None:
                            pend2[0]()
                            pend2[0] = None
                    if STAGE >= 3.15:
                        fw.dma("sp", "ascr_w", [bset_a[s]], [b_ascr], ascr_d[u], av)

                if STAGE < 4:
                    continue
                wov = wo_d[i].rearrange("(kc p) n -> p kc n", p=128)
                asv = ascr_d.rearrange("u p t -> p u t")
                lnp = LNPipe(i, 0)
                for g in range(ngr):
                    t0, n = GROUPS[g]
                    r = rowof(g)
                    hbt, bhbt, hk = rot_hb.next()
                    fw.dma("sp", f"ar{hk}", [b_ascr], [bhbt], hbt[:, :, 0:n], asv[:, :, t0:t0 + n])
                    pcs = [wload(wov[:, 2 * q:2 * q + 2, :], 2, 1024) for q in range(4)]
                    for oc in range(KC):
                        bank = rotS.next()

                        def _mm():
                            ins = None
                            for kc in range(KC):
                                ins = nc.tensor.matmul(bank[0][:, 0:n], pcs[kc // 2][0][:, kc % 2, oc * 128:(oc + 1) * 128],
                                                       hbt[:, kc, 0:n], start=(kc == 0), stop=(kc == KC - 1))
                            return ins
                        op("pe", [bhbt] + [p[1] for p in pcs], [bank[1]], _mm)
                        op("dve", [bank[1], bx[g], bmod], [bx[g]],
                           lambda: nc.vector.scalar_tensor_tensor(xT[:, oc, t0:t0 + n], bank[0][:, 0:n],
                                                                  modcol(i, 2, oc, r), xT[:, oc, t0:t0 + n],
                                                                  op0=ALU.mult, op1=ALU.add))
                    lnp.push(g)
                lnp.flush()

                if STAGE < 5 and i == NL - 1:
                    continue
                moe = not isA
                fw.fence(bset + bset_a, bh)
                for g in range(ngr):
                    t0, n = GROUPS[g]
                    r = rowof(g)
                    ntile = n // 128
                    lgp = rotA.next() if moe else None
                    for kc in range(KC):
                        h = rot_h32.next()
                        op("dve", [bx[g], bmod], [h[1]],
                           lambda: nc.vector.tensor_scalar(h[0][:, 0:n], xT[:, kc, t0:t0 + n],
                                                           modcol(i, 4, kc, r), modcol(i, 3, kc, r),
                                                           op0=ALU.mult, op1=ALU.add))
                        op("act", [h[1]], [bh[g]], lambda: nc.scalar.copy(hT(kc, t0, n), h[0][:, 0:n]))
                        if moe:
                            def _r():
                                ins = None
                                for tt in range(ntile):
                                    ins = nc.tensor.matmul(lgp[0][:, tt * 8:(tt + 1) * 8], h[0][:, tt * 128:(tt + 1) * 128],
                                                           routT[:, (j * 8 + kc) * 8:(j * 8 + kc + 1) * 8],
                                                           start=(kc == 0 and tt == 0), stop=(kc == KC - 1))
                                return ins
                            op("pe", [h[1], brout], [lgp[1]], _r)
                    if moe:
                        op("act", [lgp[1]], [blg], lambda: nc.scalar.copy(lg[:, 0:ntile * 8], lgp[0][:, 0:ntile * 8]))
                        def Lt(tt):
                            return lg[:, tt * 8:(tt + 1) * 8]

                        def R(tt, a, b_):
                            return rt[:, tt * 48 + a: tt * 48 + b_]
                        tiles = list(range(ntile))
                        for tt in tiles:
                            op("dve", [blg], [brt[tt]], lambda: nc.vector.max(out=R(tt, 0, 8), in_=Lt(tt)))
                        for tt in tiles:
                            def _b():
                                nc.vector.tensor_scalar(R(tt, 8, 9), R(tt, 0, 1), -1.0, None, op0=ALU.mult)
                                return nc.vector.tensor_scalar(R(tt, 16, 24), Lt(tt), R(tt, 1, 2), None, op0=ALU.is_ge)
                            op("dve", [blg, brt[tt]], [brt[tt]], _b)
                        for tt in tiles:
                            op("act", [blg, brt[tt]], [brt[tt]],
                               lambda: nc.scalar.activation(R(tt, 24, 32), Lt(tt), AF.Exp, bias=R(tt, 8, 9), scale=1.0))
                        for tt in tiles:
                            op("dve", [brt[tt]], [brt[tt]],
                               lambda: nc.vector.tensor_tensor(R(tt, 32, 40), R(tt, 24, 32), R(tt, 16, 24), ALU.mult))
                        for tt in tiles:
                            op("dve", [brt[tt]], [brt[tt]], lambda: nc.vector.reduce_sum(R(tt, 40, 41), R(tt, 32, 40), AX.X))
                        for tt in tiles:
                            op("dve", [brt[tt]], [brt[tt]], lambda: nc.vector.reciprocal(R(tt, 41, 42), R(tt, 40, 41)))
                        for tt in tiles:
                            gi = (t0 // 128 + tt) * 8
                            op("dve", [brt[tt]], [bgates],
                               lambda: nc.vector.tensor_scalar(gates[:, gi:gi + 8], R(tt, 32, 40), R(tt, 41, 42), None,
                                                               op0=ALU.mult))
                if moe:
                    experts = list(range(NE))
                    nchunks = D_FFE // 128
                else:
                    experts = [None]
                    nchunks = D_FF // 128
                units = [(c, min(4, nchunks - c)) for c in range(0, nchunks, 4)]
                glist = list(range(ngr))
                for e in experts:
                    if moe:
                        Wg, Wu, Wd = mog_d[j, e], mou_d[j, e], mod_d[j, e]
                        for g in glist:
                            t0, n = GROUPS[g]
                            bank = rotS.next()
                            for tt in range(n // 128):
                                gi = (t0 // 128 + tt) * 8 + e
                                op("dve", [bgates], [bgbt],
                                   lambda: nc.vector.tensor_copy(gbt[:], gates[:, gi:gi + 1].to_broadcast([128, 128])))
                                op("pe", [bgbt, bident], [bank[1]],
                                   lambda: nc.tensor.matmul(bank[0][:, tt * 128:(tt + 1) * 128], gbt[:], ident[:],
                                                            start=True, stop=True))
                            op("act", [bank[1]], [bgbc], lambda: nc.scalar.copy(gbc[:, t0:t0 + n], bank[0][:, 0:n]))
                    else:
                        Wg, Wu, Wd = ffg_d[j], ffu_d[j], ffd_d[j]
                    Wgv = Wg.rearrange("(kc p) n -> p kc n", p=128)
                    Wuv = Wu.rearrange("(kc p) n -> p kc n", p=128)
                    Wdv = Wd.rearrange("(c p) n -> p c n", p=128)
                    for (c0, nch) in units:
                        halves = (nch + 1) // 2
                        wg = [wload(Wgv[:, :, (c0 + 2 * h_) * 128:(c0 + 2 * h_ + 2) * 128], 8, 256) for h_ in range(halves)]
                        wu = [wload(Wuv[:, :, (c0 + 2 * h_) * 128:(c0 + 2 * h_ + 2) * 128], 8, 256) for h_ in range(halves)]
                        wd = [wload(Wdv[:, c0 + 2 * h_: c0 + 2 * h_ + 2, :], 2, 1024) for h_ in range(halves)]
                        pending = None
                        for g in glist + [None]:
                            actb = None
                            if g is not None:
                                t0, n = GROUPS[g]
                                actb = rot_hb.next()
                                for c in range(nch):
                                    gb = rotS.next()
                                    ub = rotS.next()

                                    def _mm():
                                        ins = None
                                        for kc in range(KC):
                                            nc.tensor.matmul(gb[0][:, 0:n], wg[c // 2][0][:, kc, (c % 2) * 128:(c % 2 + 1) * 128],
                                                             hT(kc, t0, n), start=(kc == 0), stop=(kc == KC - 1))
                                        for kc in range(KC):
                                            ins = nc.tensor.matmul(ub[0][:, 0:n], wu[c // 2][0][:, kc, (c % 2) * 128:(c % 2 + 1) * 128],
                                                                   hT(kc, t0, n), start=(kc == 0), stop=(kc == KC - 1))
                                        return ins
                                    op("pe", [bh[g], wg[c // 2][1], wu[c // 2][1]], [gb[1], ub[1]], _mm)
                                    st = rot_st.next()
                                    op("act", [gb[1]], [st[1]],
                                       lambda: nc.scalar.activation(st[0][:, 0:n], gb[0][:, 0:n], AF.Silu))
                                    if moe:
                                        op("dve", [st[1], ub[1]], [st[1]],
                                           lambda: nc.vector.tensor_tensor(st[0][:, 0:n], st[0][:, 0:n], ub[0][:, 0:n], ALU.mult))
                                        op("dve", [st[1], bgbc], [actb[1]],
                                           lambda: nc.vector.tensor_tensor(actb[0][:, c, 0:n], st[0][:, 0:n],
                                                                           gbc[:, t0:t0 + n], ALU.mult))
                                    else:
                                        op("dve", [st[1], ub[1]], [actb[1]],
                                           lambda: nc.vector.tensor_tensor(actb[0][:, c, 0:n], st[0][:, 0:n],
                                                                           ub[0][:, 0:n], ALU.mult))
                            if pending is not None:
                                pg, pact = pending
                                pt0, pn = GROUPS[pg]
                                pr = rowof(pg)
                                for oc in range(KC):
                                    bank = rotA.next()

                                    def _dn():
                                        ins = None
                                        for c in range(nch):
                                            ins = nc.tensor.matmul(bank[0][:, 0:pn],
                                                                   wd[c // 2][0][:, c % 2, oc * 128:(oc + 1) * 128],
                                                                   pact[0][:, c, 0:pn], start=(c == 0), stop=(c == nch - 1))
                                        return ins
                                    op("pe", [pact[1]] + [w_[1] for w_ in wd], [bank[1]], _dn)
                                    op("dve", [bank[1], bx[pg], bmod], [bx[pg]],
                                       lambda: nc.vector.scalar_tensor_tensor(xT[:, oc, pt0:pt0 + pn], bank[0][:, 0:pn],
                                                                              modcol(i, 5, oc, pr), xT[:, oc, pt0:pt0 + pn],
                                                                              op0=ALU.mult, op1=ALU.add))
                            pending = (g, actb) if g is not None else None
                lnp = LNPipe(i, 1)
                for g in glist:
                    lnp.push(g)
                lnp.flush()

            ov = outT_d[bi].rearrange("(kc p) t -> p kc t", p=128)
            for g in range(4):
                t0, n = GROUPS[g]
                fw.dma("sp", "out", [bx[g]], [], ov[:, :, t0:t0 + n], xT[:, :, t0:t0 + n])
        so = fw.dsem["out"]
        nc.sync.wait_ge(so[0], so[1])
        print("build: cnt", fw.cnt, "waits", fw.nwait)
    return nc


def _rope_tables():
    half = 32
    inv = 10000.0 ** (-np.arange(0, half, 2, dtype=np.float32) / half)
    t = np.arange(LAT)
    row = (t // 64).astype(np.float32)
    col = (t % 64).astype(np.float32)
    ang_r = row[:, None] * inv[None, :]
    ang_c = col[:, None] * inv[None, :]
    ang = np.concatenate([ang_r, ang_r, ang_c, ang_c], axis=-1).astype(np.float32)
    cos = np.cos(ang).astype(np.float32)
    sin = np.sin(ang).astype(np.float32)
    sign = np.concatenate([-np.ones(16), np.ones(16), -np.ones(16), np.ones(16)]).astype(np.float32)
    cosT = np.ones((128, TOK), np.float32)
    sinT = np.zeros((128, TOK), np.float32)
    cosT[0:64, 0:LAT] = cos.T
    cosT[64:128, 0:LAT] = cos.T
    sinT[0:64, 0:LAT] = (sin * sign[None, :]).T
    sinT[64:128, 0:LAT] = (sin * sign[None, :]).T
    return cosT, sinT


def _perm128():
    p64 = np.concatenate([np.arange(16, 32), np.arange(0, 16), np.arange(48, 64), np.arange(32, 48)])
    return np.concatenate([p64, 64 + p64])


def _colT(v):
    v = np.asarray(v, np.float32)
    lead = v.shape[:-1]
    n = v.shape[-1] // 128
    a = v.reshape(lead + (n, 128))
    a = np.moveaxis(a, -1, 0)
    return np.ascontiguousarray(a.reshape(128, -1))


def prepare_shared(inp):
    f = lambda k: np.asarray(inp[k], np.float32)
    perm = _perm128()
    a_w = f("a_w_qkv")
    b_w = f("b_w_qkv")
    wattn = np.zeros((DEPTH, 8, D, 640), np.float32)
    for i in range(DEPTH):
        j = i // 2
        for u in range(8):
            if i % 2 == 0:
                W = a_w[j]
                q = W[:, 128 * u:128 * u + 128]
                g = u // 2
                kc = W[:, 1024 + 64 * g: 1024 + 64 * g + 64]
                k = np.concatenate([kc, kc], axis=1)
                v = np.zeros((D, 128), np.float32)
                v[:, 0:64] = W[:, 1280 + 64 * g: 1280 + 64 * g + 64]
            else:
                W = b_w[j]
                q = W[:, 128 * u:128 * u + 128]
                k = W[:, 1024 + 128 * u: 1024 + 128 * u + 128]
                v = W[:, 2048 + 128 * u: 2048 + 128 * u + 128]
            wattn[i, u, :, 0:128] = q
            wattn[i, u, :, 128:256] = q[:, perm]
            wattn[i, u, :, 256:384] = k
            wattn[i, u, :, 384:512] = k[:, perm]
            wattn[i, u, :, 512:640] = v
    wo = np.zeros((DEPTH, D, D), np.float32)
    a_o = f("a_w_o")
    b_o = f("b_w_o")
    for i in range(DEPTH):
        wo[i] = a_o[i // 2] if i % 2 == 0 else b_o[i // 2]
    cosT, sinT = _rope_tables()
    jj = np.arange(128)[:, None]
    ii = np.arange(128)[None, :]
    mprev = (jj >= ii).astype(np.float32)
    mnext = (jj <= ii).astype(np.float32)
    masks = np.concatenate([mprev, mprev, mnext, mnext], axis=1)
    lam = np.stack([np.stack([f("b_lam_q1")[j], f("b_lam_k1")[j], f("b_lam_q2")[j], f("b_lam_k2")[j]]) for j in range(2)])
    lamv = np.ascontiguousarray(np.broadcast_to(lam.reshape(1, -1), (128, 512))).astype(np.float32)
    rout = f("moe_w_router")
    routT = np.ascontiguousarray(rout.reshape(2, KC, 128, NE).transpose(2, 0, 1, 3).reshape(128, -1))
    shared = {
        "w_mod": f("w_mod"),
        "bmodT": _colT(f("b_mod")),
        "lngT": _colT(f("ln_g")),
        "lnbT": _colT(f("ln_b")),
        "wattn": wattn,
        "wo": wo,
        "ffg": f("ff_w_gate"), "ffu": f("ff_w_up"), "ffd": f("ff_w_down"),
        "mog": f("moe_w_gate"), "mou": f("moe_w_up"), "mod": f("moe_w_down"),
        "routT": routT,
        "cosT": cosT, "sinT": sinT, "masks": masks,
        "sink": np.ascontiguousarray(f("a_sink").reshape(1, 32)),
        "lamv": lamv,
        "sublnT": np.ascontiguousarray(f("b_subln_g").T),
        "ident": np.eye(128, dtype=np.float32),
    }
    return shared


def prepare_core(inp, batches):
    x = np.asarray(inp["x"], np.float32)
    ctx = np.asarray(inp["ctx"], np.float32)
    c = np.asarray(inp["c"], np.float32)
    c_ctx = np.asarray(inp["c_ctx"], np.float32)
    xT = np.empty((len(batches), D, TOK), np.float32)
    for k, b in enumerate(batches):
        xT[k, :, 0:LAT] = x[b].T
        xT[k, :, LAT:] = ctx[b].T
    rows = [c[b] for b in batches]
    while len(rows) < 2:
        rows.append(c[batches[0]])
    rows.append(c_ctx)
    rows = np.stack(rows)
    cT = np.ascontiguousarray(rows.reshape(3, KC, 128).transpose(2, 1, 0).reshape(128, KC * 3))
    return {"xT": xT, "cT": cT}


_NC_CACHE = {}


def kernel(**inputs):
    ncores = 8
    shared = prepare_shared(inputs)
    in_maps = []
    for cidx in range(ncores):
        m = dict(shared)
        m.update(prepare_core(inputs, [NB * cidx + k for k in range(NB)]))
        in_maps.append(m)
    if "nc" not in _NC_CACHE:
        _NC_CACHE["nc"] = build_nc()
    res = run_bass_kernel_spmd(_NC_CACHE["nc"], in_maps, core_ids=list(range(ncores)))
    out = np.empty((ncores * NB, LAT, D), np.float32)
    for cidx in range(ncores):
        oT = res.results[cidx]["outT"]
        for k in range(NB):
            out[NB * cidx + k] = oT[k].T
    return out
```
